# Optimizing a Trainium2 kernel written in Bass

```python
import math
import jax, jax.numpy as jnp
from jax import lax
import numpy as np

D_MODEL = 1024
BATCH = 8
SEQ = 4096
DEPTH = 2

MLA_HEADS = 8
MLA_Q_LORA = 384
MLA_KV_LORA = 256
MLA_DN = 64
MLA_DR = 32
MLA_DV = 64
ROPE_THETA = 10000.0
Q_BLOCK = 128
SSM_HEADS = 16
SSM_HEAD_DIM = 64
SSM_D_INNER = SSM_HEADS * SSM_HEAD_DIM
SSM_GROUPS = 4
SSM_STATE = 64
SSM_CONV = 5
SSM_CHUNK = 128
SSM_CONV_DIM = SSM_D_INNER + 2 * SSM_GROUPS * SSM_STATE
CNV_CH = 512
CNV_WIDTH = 31
N_EXPERTS = 32
TOP_K = 4
D_FF = 1024
SWIGLU_LIMIT = 7.0
SWIGLU_ALPHA = 1.702
MOE_BLOCK = 128
DN_ALPHA = (2 * DEPTH) ** 0.25
DN_BETA = (8 * DEPTH) ** -0.25
IN_SIZES = (MLA_Q_LORA, MLA_KV_LORA, MLA_DR,
            SSM_D_INNER, SSM_CONV_DIM, 2 * SSM_HEADS,
            CNV_CH, CNV_CH,
            3 * D_MODEL)
N_IN = sum(IN_SIZES)

kernel_name = "hybrid_mla_ssd_conformer_moe_deepnorm"


def _split(t, sizes):
    offs = np.cumsum(sizes)[:-1].tolist()
    return jnp.split(t, offs, axis=-1)


def _layernorm(x, g, b, eps=1e-5):
    xf = x.astype(jnp.float32)
    mu = jnp.mean(xf, -1, keepdims=True)
    var = jnp.mean(jnp.square(xf - mu), -1, keepdims=True)
    return ((xf - mu) * lax.rsqrt(var + eps) * g + b).astype(x.dtype)


def _rmsnorm(x, g, eps=1e-6):
    xf = x.astype(jnp.float32)
    return (xf * lax.rsqrt(jnp.mean(xf * xf, -1, keepdims=True) + eps) * g).astype(x.dtype)


def _rope_tables(seq):
    pos = jnp.arange(seq, dtype=jnp.float32)
    inv = ROPE_THETA ** (-jnp.arange(0, MLA_DR, 2, dtype=jnp.float32) / MLA_DR)
    ang = pos[:, None] * inv[None, :]
    return jnp.cos(ang), jnp.sin(ang)


def _apply_rope(t, cos, sin):
    t1, t2 = jnp.split(t, 2, axis=-1)
    return jnp.concatenate([t1 * cos - t2 * sin, t1 * sin + t2 * cos], -1).astype(t.dtype)


def _dwconv(x, w, b):
    width = w.shape[0]
    y = lax.conv_general_dilated(x, w[:, None, :].astype(x.dtype), window_strides=(1,),
                                 padding=[((width - 1) // 2, width // 2)],
                                 dimension_numbers=('NWC', 'WIO', 'NWC'),
                                 feature_group_count=x.shape[-1])
    return y + b


def _mla(c_q, c_kv, k_rope, q_norm, kv_norm, w_uq, w_ukv, cos, sin):
    bsz, s, _ = c_q.shape
    q = (_rmsnorm(c_q, q_norm) @ w_uq).reshape(bsz, s, MLA_HEADS, MLA_DN + MLA_DR)
    q_nope = q[..., :MLA_DN]
    q_rope = _apply_rope(q[..., MLA_DN:], cos[:, None, :], sin[:, None, :])
    kv = (_rmsnorm(c_kv, kv_norm) @ w_ukv).reshape(bsz, s, MLA_HEADS, MLA_DN + MLA_DV)
    k_nope, v = kv[..., :MLA_DN], kv[..., MLA_DN:]
    k_rope = _apply_rope(k_rope, cos, sin)
    scale = (MLA_DN + MLA_DR) ** -0.5
    nb = s // Q_BLOCK
    qn = q_nope.reshape(bsz, nb, Q_BLOCK, MLA_HEADS, MLA_DN).transpose(1, 0, 2, 3, 4)
    qr = q_rope.reshape(bsz, nb, Q_BLOCK, MLA_HEADS, MLA_DR).transpose(1, 0, 2, 3, 4)

    def block(args):
        qn_b, qr_b = args
        sc = (jnp.einsum('bqhd,bkhd->bhqk', qn_b, k_nope)
              + jnp.einsum('bqhd,bkd->bhqk', qr_b, k_rope))
        p = jax.nn.softmax(sc.astype(jnp.float32) * scale, axis=-1).astype(v.dtype)
        return jnp.einsum('bhqk,bkhd->bqhd', p, v)

    o = lax.map(block, (qn, qr))
    return o.transpose(1, 0, 2, 3, 4).reshape(bsz, s, MLA_HEADS * MLA_DV)


def _ssd(xh, dta, bm, cm):
    bsz, s, g, r, p = xh.shape
    nc, l = s // SSM_CHUNK, SSM_CHUNK
    x = xh.reshape(bsz, nc, l, g, r, p)
    bc = bm.reshape(bsz, nc, l, g, -1)
    cc = cm.reshape(bsz, nc, l, g, -1)
    a = dta.astype(jnp.float32).reshape(bsz, nc, l, g, r).transpose(0, 3, 4, 1, 2)
    a_cs = jnp.cumsum(a, axis=-1)
    seg = a_cs[..., :, None] - a_cs[..., None, :]
    lower = jnp.tril(jnp.ones((l, l), dtype=bool))
    decay = jnp.exp(jnp.where(lower, seg, -jnp.inf)).astype(x.dtype)
    cb = jnp.einsum('bclgn,bcsgn->bcgls', cc, bc)
    y_diag = jnp.einsum('bcgls,bgrcls,bcsgrp->bclgrp', cb, decay, x)
    decay_states = jnp.exp(a_cs[..., -1:] - a_cs).astype(x.dtype)
    states = jnp.einsum('bclgn,bgrcl,bclgrp->bcgrpn', bc, decay_states, x)
    chunk_decay = jnp.exp(a_cs[..., -1])

    def step(h, inp):
        st, dec = inp
        return h * dec[..., None, None] + st, h

    h0 = jnp.zeros(states.shape[:1] + states.shape[2:], jnp.float32)
    _, h_in = lax.scan(step, h0, (states.transpose(1, 0, 2, 3, 4, 5).astype(jnp.float32),
                                  chunk_decay.transpose(3, 0, 1, 2)))
    h_in = h_in.transpose(1, 0, 2, 3, 4, 5).astype(x.dtype)
    y_off = jnp.einsum('bclgn,bcgrpn,bgrcl->bclgrp', cc, h_in, jnp.exp(a_cs).astype(x.dtype))
    return (y_diag + y_off).reshape(bsz, s, g, r, p)


def _mamba2(z, xbc, dt_raw, conv_w, conv_b, dt_bias, a_log, d_skip, norm_g):
    bsz, s, _ = z.shape
    g, r = SSM_GROUPS, SSM_HEADS // SSM_GROUPS
    xbc = jax.nn.silu(_dwconv(xbc, conv_w, conv_b))
    xs, bm, cm = _split(xbc, (SSM_D_INNER, SSM_GROUPS * SSM_STATE, SSM_GROUPS * SSM_STATE))
    xh = xs.reshape(bsz, s, g, r, SSM_HEAD_DIM)
    bm = bm.reshape(bsz, s, g, SSM_STATE)
    cm = cm.reshape(bsz, s, g, SSM_STATE)
    dt = jax.nn.softplus(dt_raw.reshape(bsz, s, 2, SSM_HEADS) + dt_bias)
    dta = dt * (-jnp.exp(a_log))

    def direction(i, flip):
        f = (lambda t: jnp.flip(t, axis=1)) if flip else (lambda t: t)
        dt_i = dt[:, :, i].reshape(bsz, s, g, r)
        y = _ssd(f(xh * dt_i[..., None]), f(dta[:, :, i].reshape(bsz, s, g, r)), f(bm), f(cm))
        return f(y)

    y = direction(0, False) + direction(1, True) + xh * d_skip.reshape(g, r)[..., None]
    y = y.reshape(bsz, s, SSM_D_INNER) * jax.nn.silu(z)
    y = _rmsnorm(y.reshape(bsz, s, g, -1), norm_g.reshape(g, -1))
    return y.reshape(bsz, s, SSM_D_INNER)


def _conformer_conv(a, gt, dw_w, dw_b, ln_g, ln_b):
    u = a * jax.nn.sigmoid(gt)
    u = _dwconv(u, dw_w, dw_b)
    return jax.nn.silu(_layernorm(u, ln_g, ln_b))


def _moe(t, router_w, router_b, w_gu, b_gu, w_dn, b_dn):
    n_tok, d = t.shape
    logits = (t @ router_w + router_b).astype(jnp.float32)
    top_vals, top_idx = lax.top_k(logits, TOP_K)
    gates = jax.nn.softmax(top_vals, axis=-1).astype(t.dtype)
    tk = n_tok * TOP_K
    n_blocks = -(-(tk + N_EXPERTS * (MOE_BLOCK - 1)) // MOE_BLOCK)
    n_rows = n_blocks * MOE_BLOCK
    flat_e = top_idx.reshape(tk).astype(jnp.int32)
    flat_tok = jnp.repeat(jnp.arange(n_tok, dtype=jnp.int32), TOP_K)
    flat_gate = gates.reshape(tk)
    order = jnp.argsort(flat_e)
    se = flat_e[order]
    counts = jnp.bincount(flat_e, length=N_EXPERTS).astype(jnp.int32)
    padded = (counts + MOE_BLOCK - 1) // MOE_BLOCK * MOE_BLOCK
    pad_end = jnp.cumsum(padded)
    pad_start = pad_end - padded
    start = jnp.cumsum(counts) - counts
    dest = pad_start[se] + jnp.arange(tk, dtype=jnp.int32) - start[se]
    row_tok = jnp.zeros((n_rows,), jnp.int32).at[dest].set(flat_tok[order])
    row_gate = jnp.zeros((n_rows,), t.dtype).at[dest].set(flat_gate[order])
    blk_e = jnp.minimum(jnp.searchsorted(pad_end, jnp.arange(n_blocks, dtype=jnp.int32) * MOE_BLOCK,
                                         side='right'), N_EXPERTS - 1)
    xb = t[row_tok].reshape(n_blocks, MOE_BLOCK, d)

    def expert_block(args):
        xe, e = args
        h = xe @ w_gu[e] + b_gu[e]
        gate, up = h[:, :D_FF], h[:, D_FF:]
        gate = jnp.minimum(gate, SWIGLU_LIMIT)
        up = jnp.clip(up, -SWIGLU_LIMIT, SWIGLU_LIMIT)
        return ((up + 1) * (gate * jax.nn.sigmoid(SWIGLU_ALPHA * gate))) @ w_dn[e] + b_dn[e]

    yb = lax.map(expert_block, (xb, blk_e)).reshape(n_rows, d)
    return jnp.zeros_like(t).at[row_tok].add(yb * row_gate[:, None])


def setup_inputs(seed: int = 0) -> dict:
    key = jax.random.key(seed)
    ks = iter(jax.random.split(key, 48))
    L = DEPTH
    f32 = jnp.float32

    def nrm(shape, fan_in, scale=1.0):
        return jax.random.normal(next(ks), shape, f32) * (scale * fan_in ** -0.5)

    def gain(shape):
        return 1.0 + 0.02 * jax.random.normal(next(ks), shape, f32)

    def bias(shape, scale=0.02):
        return scale * jax.random.normal(next(ks), shape, f32)

    x = jax.random.normal(next(ks), (BATCH, SEQ, D_MODEL), f32)
    dt0 = jnp.exp(jax.random.uniform(next(ks), (L, 2, SSM_HEADS), f32,
                                     math.log(1e-3), math.log(1e-1)))
    dt_bias = dt0 + jnp.log(-jnp.expm1(-dt0))
    a_log = jnp.log(jax.random.uniform(next(ks), (L, 2, SSM_HEADS), f32, 1.0, 16.0))
    return {
        "x": x,
        "w_in": nrm((L, D_MODEL, N_IN), D_MODEL),
        "b_in": bias((L, N_IN)),
        "mla_q_norm": gain((L, MLA_Q_LORA)),
        "mla_kv_norm": gain((L, MLA_KV_LORA)),
        "mla_w_uq": nrm((L, MLA_Q_LORA, MLA_HEADS * (MLA_DN + MLA_DR)), MLA_Q_LORA),
        "mla_w_ukv": nrm((L, MLA_KV_LORA, MLA_HEADS * (MLA_DN + MLA_DV)), MLA_KV_LORA),
        "w_br_attn": nrm((L, MLA_HEADS * MLA_DV, D_MODEL), MLA_HEADS * MLA_DV, DN_BETA),
        "ssm_conv_w": nrm((L, SSM_CONV, SSM_CONV_DIM), SSM_CONV),
        "ssm_conv_b": bias((L, SSM_CONV_DIM)),
        "ssm_dt_bias": dt_bias,
        "ssm_a_log": a_log,
        "ssm_d": 1.0 + 0.1 * jax.random.normal(next(ks), (L, SSM_HEADS), f32),
        "ssm_norm": gain((L, SSM_D_INNER)),
        "w_br_ssm": nrm((L, SSM_D_INNER, D_MODEL), SSM_D_INNER, DN_BETA),
        "cnv_dw_w": nrm((L, CNV_WIDTH, CNV_CH), CNV_WIDTH),
        "cnv_dw_b": bias((L, CNV_CH)),
        "cnv_ln_g": gain((L, CNV_CH)),
        "cnv_ln_b": bias((L, CNV_CH)),
        "w_br_conv": nrm((L, CNV_CH, D_MODEL), CNV_CH, DN_BETA),
        "b_br_conv": bias((L, D_MODEL)),
        "w_out": nrm((L, D_MODEL, D_MODEL), D_MODEL, DN_BETA),
        "ln1_g": gain((L, D_MODEL)),
        "ln1_b": bias((L, D_MODEL)),
        "router_w": nrm((L, D_MODEL, N_EXPERTS), D_MODEL),
        "router_b": bias((L, N_EXPERTS), 0.01),
        "moe_w_gate_up": nrm((L, N_EXPERTS, D_MODEL, 2 * D_FF), D_MODEL),
        "moe_b_gate_up": bias((L, N_EXPERTS, 2 * D_FF)),
        "moe_w_down": nrm((L, N_EXPERTS, D_FF, D_MODEL), D_FF, DN_BETA),
        "moe_b_down": bias((L, N_EXPERTS, D_MODEL)),
        "ln2_g": gain((L, D_MODEL)),
        "ln2_b": bias((L, D_MODEL)),
    }


def reference(x, w_in, b_in, mla_q_norm, mla_kv_norm, mla_w_uq, mla_w_ukv, w_br_attn,
              ssm_conv_w, ssm_conv_b, ssm_dt_bias, ssm_a_log, ssm_d, ssm_norm, w_br_ssm,
              cnv_dw_w, cnv_dw_b, cnv_ln_g, cnv_ln_b, w_br_conv, b_br_conv,
              w_out, ln1_g, ln1_b, router_w, router_b, moe_w_gate_up, moe_b_gate_up,
              moe_w_down, moe_b_down, ln2_g, ln2_b):
    bsz, s, d = x.shape
    cos, sin = _rope_tables(s)
    for l in range(DEPTH):
        proj = x @ w_in[l] + b_in[l]
        (c_q, c_kv, k_rope, z, xbc, dt_raw, cnv_a, cnv_g, gate_logits) = _split(proj, IN_SIZES)
        y_attn = _mla(c_q, c_kv, k_rope, mla_q_norm[l], mla_kv_norm[l], mla_w_uq[l],
                      mla_w_ukv[l], cos, sin) @ w_br_attn[l]
        y_ssm = _mamba2(z, xbc, dt_raw, ssm_conv_w[l], ssm_conv_b[l], ssm_dt_bias[l],
                        ssm_a_log[l], ssm_d[l], ssm_norm[l]) @ w_br_ssm[l]
        y_conv = _conformer_conv(cnv_a, cnv_g, cnv_dw_w[l], cnv_dw_b[l], cnv_ln_g[l],
                                 cnv_ln_b[l]) @ w_br_conv[l] + b_br_conv[l]
        g_attn, g_ssm, g_conv = jnp.split(jax.nn.sigmoid(gate_logits), 3, axis=-1)
        mixed = (g_attn * y_attn + g_ssm * y_ssm + g_conv * y_conv) @ w_out[l]
        x = _layernorm(DN_ALPHA * x + mixed, ln1_g[l], ln1_b[l])
        ffn = _moe(x.reshape(bsz * s, d), router_w[l], router_b[l], moe_w_gate_up[l],
                   moe_b_gate_up[l], moe_w_down[l], moe_b_down[l]).reshape(bsz, s, d)
        x = _layernorm(DN_ALPHA * x + ffn, ln2_g[l], ln2_b[l])
    return x
```

```python
import contextlib
import numpy as np
import concourse.bass as bass
import concourse.mybir as mybir
from concourse.bass_utils import run_bass_kernel_spmd

F32 = mybir.dt.float32
BF16 = mybir.dt.bfloat16
I32 = mybir.dt.int32
U32 = mybir.dt.uint32
AF = mybir.ActivationFunctionType
ALU = mybir.AluOpType
AX = mybir.AxisListType

D = 1024
DEPTH = 2
HEADS = 8
QL = 384
KVL = 256
DN = 64
DR = 32
DV = 64
SH = 16
SP = 64
DI = 1024
SG = 4
SN = 64
SCONV = 5
CDIM = 1536
CCH = 512
CW = 31
NE = 32
TOPK = 4
DFF = 1024
LIMIT = 7.0
SALPHA = 1.702
DN_ALPHA = (2 * DEPTH) ** 0.25
OFF_CQ, OFF_CKV, OFF_KR, OFF_Z, OFF_XBC, OFF_DT, OFF_CA, OFF_CG, OFF_GATE = (
    0, 384, 640, 672, 1696, 3232, 3264, 3776, 4288)
NIN = 7360


class _I:
    __slots__ = ("eng", "fn", "deps", "sig", "sigval", "dsem", "dval")

    def __init__(self, eng, fn, deps):
        self.eng = eng
        self.fn = fn
        self.deps = [d for d in deps if d is not None]
        self.sig = False
        self.sigval = 0
        self.dsem = None
        self.dval = 0


class DSem:
    def __init__(self, h):
        self.h = h
        self.count = 0


ENGS = ("pe", "act", "dve", "pool", "sp")


class Prog:
    def __init__(self, nc, stack):
        self.nc = nc
        self.esem = {e: stack.enter_context(nc.semaphore("es_" + e)) for e in ENGS}
        self.ecount = {e: 0 for e in ENGS}
        self.dsems = [DSem(stack.enter_context(nc.semaphore("ds%d" % i))) for i in range(40)]
        self.q = {e: [] for e in ENGS}
        self.used_dsems = set()
        self.regcache = {}

    def op(self, eng, fn, deps=()):
        i = _I(eng, fn, deps)
        self.q[eng].append(i)
        return i

    def dma(self, eng, out, in_, deps=(), sem=0, **kw):
        sem = sem + (20 if eng != "pool" else 0)
        ds = self.dsems[sem]
        i = _I(eng, lambda e: e.dma_start(out=out, in_=in_, **kw), deps)
        ds.count += 16
        i.dsem = ds
        i.dval = ds.count
        self.used_dsems.add(sem)
        self.q[eng].append(i)
        return i

    def idma(self, out, out_offset, in_, in_offset, deps=(), sem=0, **kw):
        ds = self.dsems[sem]
        bc = kw.pop("bounds_check", None)

        def fn(e):
            kw2 = dict(kw)
            if bc is not None:
                if bc not in self.regcache:
                    self.regcache[bc] = e.to_reg(bc)
                kw2["bounds_check"] = self.regcache[bc]
            return e.indirect_dma_start(out=out, out_offset=out_offset, in_=in_, in_offset=in_offset, **kw2)
        i = _I("pool", fn, deps)
        ds.count += 16
        i.dsem = ds
        i.dval = ds.count
        self.used_dsems.add(sem)
        self.q["pool"].append(i)
        return i

    def run(self):
        nc = self.nc
        for e in ENGS:
            for i in self.q[e]:
                for d in i.deps:
                    if d.dsem is None and (d.eng != i.eng or d.eng != "pe"):
                        d.sig = True
        for e in ENGS:
            c = self.ecount[e]
            for i in self.q[e]:
                if i.sig:
                    c += 1
                    i.sigval = c
            self.ecount[e] = c
        finals = [(ds.h, ds.count) for ds in (self.dsems[s] for s in sorted(self.used_dsems))]
        efinal = dict(self.ecount)

        def emit(eng_name, e):
            waited = {}
            for i in self.q[eng_name]:
                need = {}
                for d in i.deps:
                    if d.dsem is not None:
                        h, v = d.dsem.h, d.dval
                    else:
                        if not d.sig:
                            continue
                        h, v = self.esem[d.eng], d.sigval
                    key = id(h)
                    if key not in need or need[key][1] < v:
                        need[key] = (h, v)
                for key, (h, v) in need.items():
                    if waited.get(key, -1) >= v:
                        continue
                    waited[key] = v
                    e.wait_ge(h, v)
                ins = i.fn(e)
                if i.dsem is not None:
                    ins.then_inc(i.dsem.h, 16)
                elif i.sig:
                    ins.then_inc(self.esem[eng_name], 1)
            if eng_name == "sp":
                for h, v in finals:
                    e.wait_ge(h, v)
                for en in ("pe", "act", "dve", "pool"):
                    if efinal[en] > 0:
                        e.wait_ge(self.esem[en], efinal[en])

        with nc.allow_non_contiguous_dma(reason="small strided parameter loads"), nc.Block() as blk:
            @blk.tensor
            def _(e):
                emit("pe", e)

            @blk.scalar
            def _(e):
                emit("act", e)

            @blk.vector
            def _(e):
                emit("dve", e)

            @blk.gpsimd
            def _(e):
                emit("pool", e)

            @blk.sync
            def _(e):
                emit("sp", e)
        self.q = {e: [] for e in ENGS}
        self.used_dsems = set()
        self.regcache = {}

    def mm(self, out, lhsT, rhs, start=True, stop=True, deps=()):
        return self.op("pe", lambda e: e.matmul(out, lhsT=lhsT, rhs=rhs, start=start, stop=stop), deps)

    def tr(self, out, in_, ident, deps=()):
        return self.op("pe", lambda e: e.transpose(out, in_, ident), deps)

    def act(self, out, in_, func, bias=None, scale=1.0, deps=(), accum_out=None):
        kw = {}
        if bias is not None:
            kw["bias"] = bias
        if accum_out is not None:
            kw["accum_out"] = accum_out
        return self.op("act", lambda e: e.activation(out=out, in_=in_, func=func, scale=scale, **kw), deps)

    def tt(self, eng, out, in0, in1, op, deps=()):
        return self.op(eng, lambda e: e.tensor_tensor(out=out, in0=in0, in1=in1, op=op), deps)

    def ts(self, eng, out, in0, s1, s2=None, op0=ALU.mult, op1=None, deps=(), accum_out=None):
        kw = {}
        if op1 is not None:
            kw["op1"] = op1
        if accum_out is not None:
            kw["accum_out"] = accum_out
        return self.op(eng, lambda e: e.tensor_scalar(out=out, in0=in0, scalar1=s1, scalar2=s2, op0=op0, **kw), deps)

    def stt(self, out, in0, scalar, in1, op0, op1, deps=()):
        return self.op("dve", lambda e: e.scalar_tensor_tensor(out=out, in0=in0, scalar=scalar, in1=in1,
                                                               op0=op0, op1=op1), deps)

    def copy(self, eng, out, in_, deps=()):
        if eng == "act":
            return self.op("act", lambda e: e.copy(out=out, in_=in_), deps)
        return self.op(eng, lambda e: e.tensor_copy(out=out, in_=in_), deps)

    def memset(self, eng, ap, val, deps=()):
        return self.op(eng, lambda e: e.memset(ap, val), deps)


class Ring:
    def __init__(self, bufs, sem0=None):
        self.bufs = bufs
        self.n = len(bufs)
        self.k = 0
        self.readers = [[] for _ in bufs]
        self.sem0 = sem0

    def next(self):
        j = self.k % self.n
        self.k += 1
        r = self.readers[j]
        self.readers[j] = []
        return j, self.bufs[j], r

    def sem(self, j):
        return self.sem0 + j


def bcast_rows(ap1d, n, parts=128):
    return ap1d.partition_broadcast(parts)


_LAYER = [0]


class Ctx:
    pass


def setup_consts(nc, P, stack, C):
    C.ps = [stack.enter_context(nc.psum_tensor("psb%d" % i, [128, 512], F32)) for i in range(8)]
    C.ident_f = stack.enter_context(nc.sbuf_tensor("ident_f", [128, 128], F32))
    C.ident_b = stack.enter_context(nc.sbuf_tensor("ident_b", [128, 128], BF16))
    C.ones_b = stack.enter_context(nc.sbuf_tensor("ones_b", [128, 128], BF16))
    C.ones_f = stack.enter_context(nc.sbuf_tensor("ones_f", [128, 128], F32))
    C.m_le = stack.enter_context(nc.sbuf_tensor("m_le", [128, 128], F32))
    C.m_lt = stack.enter_context(nc.sbuf_tensor("m_lt", [128, 128], F32))
    C.m_ge = stack.enter_context(nc.sbuf_tensor("m_ge", [128, 128], F32))
    C.m_gt = stack.enter_context(nc.sbuf_tensor("m_gt", [128, 128], F32))
    C.m_gt_b = stack.enter_context(nc.sbuf_tensor("m_gt_b", [128, 128], BF16))
    C.m_lt_b = stack.enter_context(nc.sbuf_tensor("m_lt_b", [128, 128], BF16))
    a = P.memset("pool", C.ones_f[:], 1.0)
    b = P.memset("pool", C.ones_b[:], 1.0)
    P.op("pool", lambda e: e.affine_select(out=C.ident_f[:], in_=C.ones_f[:], pattern=[[-1, 128]],
                                           compare_op=ALU.is_equal, fill=0.0, base=0, channel_multiplier=1), [a])
    P.op("pool", lambda e: e.affine_select(out=C.ident_b[:], in_=C.ones_b[:], pattern=[[-1, 128]],
                                           compare_op=ALU.is_equal, fill=0.0, base=0, channel_multiplier=1), [b])
    P.op("pool", lambda e: e.affine_select(out=C.m_le[:], in_=C.ones_f[:], pattern=[[1, 128]],
                                           compare_op=ALU.is_ge, fill=0.0, base=0, channel_multiplier=-1), [a])
    P.op("pool", lambda e: e.affine_select(out=C.m_lt[:], in_=C.ones_f[:], pattern=[[1, 128]],
                                           compare_op=ALU.is_gt, fill=0.0, base=0, channel_multiplier=-1), [a])
    P.op("pool", lambda e: e.affine_select(out=C.m_ge[:], in_=C.ones_f[:], pattern=[[-1, 128]],
                                           compare_op=ALU.is_ge, fill=0.0, base=0, channel_multiplier=1), [a])
    P.op("pool", lambda e: e.affine_select(out=C.m_gt[:], in_=C.ones_f[:], pattern=[[-1, 128]],
                                           compare_op=ALU.is_gt, fill=0.0, base=0, channel_multiplier=1), [a])
    P.op("pool", lambda e: e.affine_select(out=C.m_gt_b[:], in_=C.ones_b[:], pattern=[[-1, 128]],
                                           compare_op=ALU.is_gt, fill=0.0, base=0, channel_multiplier=1), [b])
    P.op("pool", lambda e: e.affine_select(out=C.m_lt_b[:], in_=C.ones_b[:], pattern=[[1, 128]],
                                           compare_op=ALU.is_gt, fill=0.0, base=0, channel_multiplier=-1), [b])
    P.run()


def phase_inproj(nc, P, C, S, x, w_in, b_in, tm_q, tm_z, tm_dt, fm_xbc, fm_u, fm_g):
    with contextlib.ExitStack() as st:
        sb = lambda name, shape, dt: st.enter_context(nc.sbuf_tensor(name + "_L%d" % _LAYER[0], shape, dt))
        w = sb("p1_w", [128, 8, NIN], BF16)
        btm = sb("p1_btm", [128, 1728], F32)
        bfm = sb("p1_bfm", [128, 44], F32)
        xin_r = Ring([sb("p1_xin%d" % i, [128, D], F32) for i in range(2)], 2)
        xbf_r = Ring([sb("p1_xbf%d" % i, [128, D], BF16) for i in range(2)])
        xT_r = Ring([sb("p1_xT%d" % i, [128, 8, 512], BF16) for i in range(2)])
        otm_r = Ring([sb("p1_otm%d" % i, [128, 1728], F32) for i in range(1)], 4)
        ofm_r = Ring([sb("p1_ofm%d" % i, [128, 512], F32) for i in range(3)], 6)
        ogb_r = Ring([sb("p1_ogb%d" % i, [128, 512], BF16) for i in range(3)], 10)
        oa_r = Ring([sb("p1_oa%d" % i, [128, 512], F32) for i in range(2)])
        psr = Ring(C.ps[0:6])
        ptr = Ring(C.ps[6:8])

        wl = []
        for c in range(8):
            for j in range(4):
                wl.append(P.dma("pool", w[:, c, j * 1840:(j + 1) * 1840],
                                w_in[c * 128:(c + 1) * 128, j * 1840:(j + 1) * 1840], sem=0))
        bl = [P.dma("sp", btm[:, 0:1696], b_in[0:1696].partition_broadcast(128), sem=1),
              P.dma("sp", btm[:, 1696:1728], b_in[OFF_DT:OFF_DT + 32].partition_broadcast(128), sem=1)]
        fcols = [("x", i, OFF_XBC + 128 * i) for i in range(12)]
        for i in range(4):
            fcols += [("a", i, OFF_CA + 128 * i), ("g", i, OFF_CG + 128 * i)]
        fcols += [("s", i, OFF_GATE + 128 * i) for i in range(24)]
        for j, (_, _, co) in enumerate(fcols):
            bl.append(P.dma("sp", bfm[:, j:j + 1], b_in[co:co + 128].rearrange("(p o) -> p o", o=1), sem=1))

        nchunk = S // 512
        for ch in range(nchunk):
            jxT, xTt, xT_readers = xT_r.next()
            xT_w = []
            for tt in range(4):
                t0 = ch * 512 + tt * 128
                ji, xi, r1 = xin_r.next()
                ld = P.dma("sp", xi[:], x[t0:t0 + 128, :], deps=r1, sem=xin_r.sem(ji))
                jb, xb, r2 = xbf_r.next()
                cv = P.copy("act", xb[:], xi[:], deps=[ld] + r2)
                xin_r.readers[ji].append(cv)
                for kc in range(0, 8, 4):
                    jp, pt, r3 = ptr.next()
                    ptb = pt[:].bitcast(BF16)
                    trs = []
                    for q in range(4):
                        trs.append(P.tr(ptb[:, q * 128:(q + 1) * 128], xb[:, (kc + q) * 128:(kc + q + 1) * 128],
                                        C.ident_b[:], deps=[cv] + r3))
                    ev = P.copy("dve", xTt[:, kc:kc + 4, tt * 128:(tt + 1) * 128],
                                ptb[:, 0:512].rearrange("p (q t) -> p q t", q=4), deps=trs + xT_readers)
                    ptr.readers[jp].append(ev)
                    xbf_r.readers[jb].extend(trs)
                    xT_w.append(ev)
            rd = []
            for tt in range(4):
                t0 = ch * 512 + tt * 128
                jo, ot, r4 = otm_r.next()
                segs = [(0, 512, 0), (512, 160, 512), (OFF_Z, 512, 672), (OFF_Z + 512, 512, 1184), (OFF_DT, 32, 1696)]
                evs = []
                for (co, n, oo) in segs:
                    jp, pb, r5 = psr.next()
                    mms = []
                    for kc in range(8):
                        mms.append(P.mm(pb[:, 0:n], xTt[:, kc, tt * 128:(tt + 1) * 128], w[:, kc, co:co + n],
                                        start=(kc == 0), stop=(kc == 7), deps=xT_w + wl + r5))
                    rd.append(mms[-1])
                    ev = P.tt("dve", ot[:, oo:oo + n], pb[:, 0:n], btm[:, oo:oo + n], ALU.add,
                              deps=[mms[-1]] + bl + r4)
                    psr.readers[jp].append(ev)
                    evs.append(ev)
                sz = P.act(ot[:, 672:1696], ot[:, 672:1696], AF.Silu, deps=[evs[2], evs[3]])
                s1 = P.dma("pool", tm_q[t0:t0 + 128, :], ot[:, 0:672], deps=[evs[0], evs[1]], sem=otm_r.sem(jo))
                s2 = P.dma("pool", tm_z[t0:t0 + 128, :], ot[:, 672:1696], deps=[sz], sem=otm_r.sem(jo))
                s3 = P.dma("pool", tm_dt[t0:t0 + 128, :], ot[:, 1696:1728], deps=[evs[4]], sem=otm_r.sem(jo))
                otm_r.readers[jo].extend([s1, s2, s3])
            ts = slice(ch * 512, (ch + 1) * 512)
            a_cur = None
            for j, (kind, i, co) in enumerate(fcols):
                jp, pb, r5 = psr.next()
                mms = []
                for kc in range(8):
                    mms.append(P.mm(pb[:, :], w[:, kc, co:co + 128], xTt[:, kc, :],
                                    start=(kc == 0), stop=(kc == 7), deps=xT_w + wl + r5))
                rd.append(mms[-1])
                bcol = bfm[:, j:j + 1]
                if kind == "x":
                    jo, o, r6 = ogb_r.next()
                    ev = P.act(o[:], pb[:, :], AF.Identity, bias=bcol, deps=[mms[-1]] + bl + r6)
                    s = P.dma("pool", fm_xbc[i * 128:(i + 1) * 128, ts], o[:], deps=[ev], sem=ogb_r.sem(jo))
                    ogb_r.readers[jo].append(s)
                elif kind == "a":
                    ja, o, r6 = oa_r.next()
                    ev = P.act(o[:], pb[:, :], AF.Identity, bias=bcol, deps=[mms[-1]] + bl + r6)
                    a_cur = (o, ev, ja)
                elif kind == "g":
                    jo, o, r6 = ofm_r.next()
                    ev0 = P.act(o[:], pb[:, :], AF.Sigmoid, bias=bcol, deps=[mms[-1]] + bl + r6)
                    ao, aev, ja = a_cur
                    jo2, o2, r7 = ogb_r.next()
                    ev = P.tt("dve", o2[:], o[:], ao[:], ALU.mult, deps=[ev0, aev] + r7)
                    oa_r.readers[ja].append(ev)
                    ofm_r.readers[jo].append(ev)
                    s = P.dma("pool", fm_u[i * 128:(i + 1) * 128, ts], o2[:], deps=[ev], sem=ogb_r.sem(jo2))
                    ogb_r.readers[jo2].append(s)
                    ev = ev0
                else:
                    jo, o, r6 = ogb_r.next()
                    ev = P.act(o[:], pb[:, :], AF.Sigmoid, bias=bcol, deps=[mms[-1]] + bl + r6)
                    s = P.dma("pool", fm_g[i * 128:(i + 1) * 128, ts], o[:], deps=[ev], sem=ogb_r.sem(jo))
                    ogb_r.readers[jo].append(s)
                psr.readers[jp].append(ev)
            xT_r.readers[jxT].extend(rd)
        P.run()


def phase_conv(nc, P, C, S, fm_xbc, fm_u, sw, sbias, cw, cbias, lng, lnb, fm_xa, fm_cv):
    NB = S // 512
    with contextlib.ExitStack() as st:
        sb = lambda name, shape, dt: st.enter_context(nc.sbuf_tensor(name + "_L%d" % _LAYER[0], shape, dt))
        wts = sb("p2_w", [128, 12, SCONV], F32)
        bts = sb("p2_b", [128, 12], F32)
        wtc = sb("p2_wc", [128, 4, CW], F32)
        btc = sb("p2_bc", [128, 4], F32)
        gl = sb("p2_g", [128, 4], F32)
        bl_ = sb("p2_bl", [128, 4], F32)
        eps = sb("p2_eps", [128, 1], F32)
        dgs = sb("p2_dgs", [128, 12 * SCONV, 128], BF16)
        dgc = sb("p2_dgc", [128, 4 * CW, 128], BF16)
        xin_r = Ring([sb("p2_x%d" % i, [128, S + 32], BF16) for i in range(2)], 2)
        ob_r = Ring([sb("p2_o%d" % i, [128, S], BF16) for i in range(2)], 4)
        uc = sb("p2_uc", [128, 4, S], F32)
        sq_r = Ring([sb("p2_sq%d" % i, [128, 512], F32) for i in range(2)])
        mean = sb("p2_mean", [128, 512], F32)
        rstd = sb("p2_rstd", [128, 512], F32)
        tmp_r = Ring([sb("p2_t%d" % i, [128, 512], F32) for i in range(2)])
        ld = []
        for c in range(12):
            ld.append(P.dma("sp", wts[:, c, :], sw[:, c * 128:(c + 1) * 128].rearrange("k c -> c k"), sem=0))
            ld.append(P.dma("sp", bts[:, c:c + 1], sbias[c * 128:(c + 1) * 128].rearrange("(p o) -> p o", o=1), sem=0))
        for c in range(4):
            ld.append(P.dma("sp", wtc[:, c, :], cw[:, c * 128:(c + 1) * 128].rearrange("k c -> c k"), sem=0))
            for (t, src) in ((btc, cbias), (gl, lng), (bl_, lnb)):
                ld.append(P.dma("sp", t[:, c:c + 1], src[c * 128:(c + 1) * 128].rearrange("(p o) -> p o", o=1), sem=0))
        ld.append(P.memset("pool", eps[:], 1e-5))
        dgw = []
        for c in range(12):
            for k in range(SCONV):
                dgw.append(P.ts("dve", dgs[:, c * SCONV + k, :], C.ident_b[:], wts[:, c, k:k + 1], None, op0=ALU.mult,
                                deps=ld))
        for c in range(4):
            for k in range(CW):
                dgw.append(P.ts("dve", dgc[:, c * CW + k, :], C.ident_b[:], wtc[:, c, k:k + 1], None, op0=ALU.mult,
                                deps=ld))
        zs = []
        for b in xin_r.bufs:
            zs.append(P.memset("pool", b[:, 0:16], 0.0))
            zs.append(P.memset("pool", b[:, 16 + S:32 + S], 0.0))
        pcv = Ring(C.ps[4:8])
        for c in range(12):
            ji, xi, r1 = xin_r.next()
            l = P.dma("sp", xi[:, 16:16 + S], fm_xbc[c * 128:(c + 1) * 128, :], deps=r1 + zs, sem=xin_r.sem(ji))
            jo, ob, r3 = ob_r.next()
            evs = []
            for blk in range(NB):
                jp, pc, rp = pcv.next()
                mm = [P.mm(pc[:, :], dgs[:, c * SCONV + k, :], xi[:, 14 + k + blk * 512:14 + k + blk * 512 + 512],
                           start=(k == 0), stop=(k == SCONV - 1), deps=[l] + dgw + rp) for k in range(SCONV)]
                ev = P.act(ob[:, blk * 512:(blk + 1) * 512], pc[:, :], AF.Silu, bias=bts[:, c:c + 1],
                           deps=[mm[-1]] + r3 + ld)
                pcv.readers[jp].append(ev)
                xin_r.readers[ji].append(mm[-1])
                evs.append(ev)
            s = P.dma("pool", fm_xa[c * 128:(c + 1) * 128, :], ob[:], deps=evs, sem=ob_r.sem(jo))
            ob_r.readers[jo].append(s)
        xu = [sb("p2_xu%d" % i, [128, S + 32], BF16) for i in range(2)]
        xus = [xin_r.bufs[0], xin_r.bufs[1]] + xu
        lu = []
        for c in range(4):
            xi = xus[c]
            dz = []
            if c >= 2:
                dz = [P.memset("pool", xi[:, 0:16], 0.0), P.memset("pool", xi[:, 16 + S:32 + S], 0.0)]
                r1 = []
            else:
                _, _, r1 = xin_r.next()
            lu.append(P.dma("sp", xi[:, 16:16 + S], fm_u[c * 128:(c + 1) * 128, :], deps=r1 + zs + dz, sem=8 + c))
        psr = Ring(C.ps[0:4])
        last_norm = []
        for blk in range(NB):
            bs = slice(blk * 512, (blk + 1) * 512)
            ucw = []
            for c in range(4):
                xi = xus[c]
                jp, pc, rp = pcv.next()
                mm = [P.mm(pc[:, :], dgc[:, c * CW + k, :], xi[:, 1 + k + blk * 512:1 + k + blk * 512 + 512],
                           start=(k == 0), stop=(k == CW - 1), deps=[lu[c]] + dgw + rp) for k in range(CW)]
                ev = P.act(uc[:, c, bs], pc[:, :], AF.Identity, bias=btc[:, c:c + 1], deps=[mm[-1]] + ld)
                pcv.readers[jp].append(ev)
                ucw.append(ev)
            jp1, p_sum, r1 = psr.next()
            jp2, p_sq, r2 = psr.next()
            mm1 = []
            mm2 = []
            for c in range(4):
                mm1.append(P.mm(p_sum[:, :], C.ones_f[:], uc[:, c, bs], start=(c == 0), stop=(c == 3), deps=ucw + r1))
                js, sq, r3 = sq_r.next()
                sqi = P.act(sq[:], uc[:, c, bs], AF.Square, deps=ucw + r3)
                m = P.mm(p_sq[:, :], C.ones_f[:], sq[:], start=(c == 0), stop=(c == 3), deps=[sqi] + r2)
                sq_r.readers[js].append(m)
                mm2.append(m)
            e1 = P.ts("dve", mean[:], p_sum[:, :], 1.0 / CCH, None, op0=ALU.mult, deps=[mm1[-1]] + last_norm)
            psr.readers[jp1].append(e1)
            e2 = P.tt("dve", rstd[:], mean[:], mean[:], ALU.mult, deps=[e1] + last_norm)
            e3 = P.stt(rstd[:], p_sq[:, :], 1.0 / CCH, rstd[:], ALU.mult, ALU.subtract, deps=[e2, mm2[-1]])
            psr.readers[jp2].append(e3)
            e4 = P.act(rstd[:], rstd[:], AF.Sqrt, bias=eps[:, 0:1], deps=[e3])
            e5 = P.op("dve", lambda e, o=rstd: e.reciprocal(out=o[:], in_=o[:]), [e4])
            last_norm = []
            for c in range(4):
                jt, tmp, r4 = tmp_r.next()
                n1 = P.tt("dve", tmp[:], uc[:, c, bs], mean[:], ALU.subtract, deps=[e5] + r4)
                n2 = P.tt("dve", tmp[:], tmp[:], rstd[:], ALU.mult, deps=[n1])
                jo, ob, r5 = ob_r.next()
                n3 = P.act(ob[:, 0:512], tmp[:], AF.Silu, bias=bl_[:, c:c + 1], scale=gl[:, c:c + 1], deps=[n2] + r5)
                tmp_r.readers[jt].append(n3)
                s = P.dma("pool", fm_cv[c * 128:(c + 1) * 128, bs], ob[:, 0:512], deps=[n3], sem=ob_r.sem(jo))
                ob_r.readers[jo].append(s)
                last_norm.append(n2)
        P.run()


def rstd_from_ssq(P, ssq, n, eps_t, deps):
    a = P.act(ssq, ssq, AF.Sqrt, bias=eps_t, scale=1.0 / n, deps=deps)
    return P.op("dve", lambda e: e.reciprocal(out=ssq, in_=ssq), [a])


def phase_attn(nc, P, C, S, tm_q, qn_g, kvn_g, w_uq, w_ukv, ropec, ropes, ropeT, fm_o, Xg=None):
    NT = S // 128
    NQ = S // 512
    scale = float((DN + DR) ** -0.5)
    with contextlib.ExitStack() as st:
        sb = lambda name, shape, dt: st.enter_context(nc.sbuf_tensor(name + "_L%d" % _LAYER[0], shape, dt))
        wq = sb("p3_wq", [128, 3, 768], BF16)
        wqr = sb("p3_wqr", [128, 3, 768], BF16)
        wk = sb("p3_wk", [128, 2, 512], BF16)
        wv = sb("p3_wv", [128, 2, 512], BF16)
        gq = sb("p3_gq", [128, 384], F32)
        gkv = sb("p3_gkv", [128, 256], F32)
        eps = sb("p3_eps", [128, 1], F32)
        cqT = sb("p3_cqT", [128, 3, S], BF16)
        ckvT = sb("p3_ckvT", [128, 2, S], BF16)
        vaug = sb("p3_vaug", [128, NT, 8, 65], BF16)
        KT = [sb("p3_KT%d" % i, [96, S], BF16) for i in range(2)]
        QT = [sb("p3_QT%d" % i, [96, S], BF16) for i in range(2)]
        tin_r = Ring([sb("p3_tin%d" % i, [128, 672], F32) for i in range(2)], 2)
        cs_r = Ring([sb("p3_cs%d" % i, [128, 32], F32) for i in range(2)], 4)
        nb_r = Ring([sb("p3_nb%d" % i, [128, 768], BF16) for i in range(2)])
        junk = sb("p3_junk", [128, 384], F32)
        junkd = sb("p3_junkd", [128, 64], F32)
        ssq_r = Ring([sb("p3_ssq%d" % i, [128, 2], F32) for i in range(2)])
        rt_r = Ring([sb("p3_rt%d" % i, [96, 2, 512], F32) for i in range(2)], 6)
        rtmp = sb("p3_rtmp", [96, 2, 512], F32)
        pT_r = Ring([sb("p3_pT%d" % i, [128, 512], BF16) for i in range(4)])
        rs = sb("p3_rs", [65, 512], F32)
        bsb = sb("p3_bsb", [64, 512], F32)
        o_r = Ring([sb("p3_o%d" % i, [64, 512], BF16) for i in range(2)], 8)

        ld = []
        for kc in range(3):
            ld.append(P.dma("pool", wq[:, kc, :], w_uq[kc * 128:(kc + 1) * 128, :], sem=10))
        for kc in range(2):
            src = w_ukv[kc * 128:(kc + 1) * 128, :].rearrange("p (h t d) -> p h t d", t=2, d=64)
            ld.append(P.dma("pool", wk[:, kc, :].rearrange("p (h d) -> p h d", d=64), src[:, :, 0, :], sem=0))
            ld.append(P.dma("pool", wv[:, kc, :].rearrange("p (h d) -> p h d", d=64), src[:, :, 1, :], sem=0))
        ld.append(P.dma("sp", gq[:], qn_g.partition_broadcast(128), sem=1))
        ld.append(P.dma("sp", gkv[:], kvn_g.partition_broadcast(128), sem=1))
        ld.append(P.memset("pool", eps[:], 1e-6))
        z0 = P.memset("pool", wqr[:], 0.0)
        wq4 = wq[:].rearrange("p k (h d) -> p k h d", d=96)
        wqr4 = wqr[:].rearrange("p k (h d) -> p k h d", d=96)
        for kc in range(3):
            ld.append(P.ts("dve", wqr4[:, kc, :, 64:80], wq4[:, kc, :, 80:96], -1.0, None, op0=ALU.mult, deps=ld[0:3] + [z0]))
            ld.append(P.copy("dve", wqr4[:, kc, :, 80:96], wq4[:, kc, :, 64:80], deps=ld[0:3] + [z0]))
        ld.append(P.memset("pool", vaug[:, :, :, 64:65], 1.0))
        if Xg is not None:
            zt = sb("p3_zt", [128, 2, D], BF16)
            zm = P.memset("pool", zt[:], 0.0)
            xgv = Xg.rearrange("(g p) d -> p g d", p=128)
            for g0 in range(0, NSLOT // 128, 2):
                P.dma("pool", xgv[:, g0:g0 + 2, :], zt[:], deps=[zm], sem=18)

        psr = Ring(C.ps[0:4])
        ptr = Ring(C.ps[6:8])
        prep_w = []
        for t in range(NT):
            ts_ = slice(t * 128, (t + 1) * 128)
            ji, ti, r1 = tin_r.next()
            l1 = P.dma("sp", ti[:], tm_q[ts_, :], deps=r1, sem=tin_r.sem(ji))
            jc, cs, r2 = cs_r.next()
            l2 = P.dma("sp", cs[:, 0:16], ropec[ts_, :], deps=r2, sem=cs_r.sem(jc))
            l3 = P.dma("sp", cs[:, 16:32], ropes[ts_, :], deps=r2, sem=cs_r.sem(jc))
            js, ssq, r3 = ssq_r.next()
            a1 = P.act(junk[:, 0:384], ti[:, 0:384], AF.Square, accum_out=ssq[:, 0:1], deps=[l1] + r3 + ld)
            a2 = P.act(junk[:, 0:256], ti[:, 384:640], AF.Square, accum_out=ssq[:, 1:2], deps=[a1])
            b1 = rstd_from_ssq(P, ssq[:, 0:1], 384.0, eps[:, 0:1], [a1])
            b2 = rstd_from_ssq(P, ssq[:, 1:2], 256.0, eps[:, 0:1], [a2, b1])
            jn, nb, r4 = nb_r.next()
            n1 = P.stt(nb[:, 0:384], ti[:, 0:384], ssq[:, 0:1], gq[:], ALU.mult, ALU.mult, deps=[b1] + r4)
            n2 = P.stt(nb[:, 384:640], ti[:, 384:640], ssq[:, 1:2], gkv[:], ALU.mult, ALU.mult, deps=[b2] + r4)
            n3 = P.memset("pool", nb[:, 640:704], 0.0, deps=r4)
            k1, k2 = ti[:, 640:656], ti[:, 656:672]
            co, si = cs[:, 0:16], cs[:, 16:32]
            u1 = P.tt("dve", junkd[:, 0:16], k1, co, ALU.mult, deps=[l1, l2, l3, a2])
            u2 = P.tt("dve", junkd[:, 16:32], k2, si, ALU.mult, deps=[u1])
            u3 = P.tt("dve", nb[:, 704:720], junkd[:, 0:16], junkd[:, 16:32], ALU.subtract, deps=[u2] + r4)
            u4 = P.tt("dve", junkd[:, 32:48], k1, si, ALU.mult, deps=[u3])
            u5 = P.tt("dve", junkd[:, 48:64], k2, co, ALU.mult, deps=[u4])
            u6 = P.tt("dve", nb[:, 720:736], junkd[:, 32:48], junkd[:, 48:64], ALU.add, deps=[u5])
            tin_r.readers[ji].extend([n1, n2, u5])
            cs_r.readers[jc].append(u5)
            ssq_r.readers[js].extend([n1, n2])
            jp, pt, r5 = ptr.next()
            ptb = pt[:].bitcast(BF16)
            trs = []
            for q in range(3):
                trs.append(P.tr(ptb[:, q * 128:(q + 1) * 128], nb[:, q * 128:(q + 1) * 128], C.ident_b[:], deps=[n1] + r5))
            for q in range(2):
                trs.append(P.tr(ptb[:, (3 + q) * 128:(4 + q) * 128], nb[:, 384 + q * 128:384 + (q + 1) * 128],
                                C.ident_b[:], deps=[n2] + r5))
            trs.append(P.tr(ptb[0:96, 640:768], nb[:, 640:736], C.ident_b[:], deps=[u6, n3] + r5))
            e1 = P.copy("dve", cqT[:, :, ts_], ptb[:, 0:384].rearrange("p (q t) -> p q t", q=3), deps=trs)
            e2 = P.copy("dve", ckvT[:, :, ts_], ptb[:, 384:640].rearrange("p (q t) -> p q t", q=2), deps=trs)
            e3 = P.copy("dve", KT[0][64:96, ts_], ptb[64:96, 640:768], deps=trs)
            e4 = P.copy("dve", KT[1][64:96, ts_], ptb[64:96, 640:768], deps=trs)
            ptr.readers[jp].extend([e1, e2, e3, e4])
            nb_r.readers[jn].extend(trs)
            jv, pv, r6 = psr.next()
            mv = []
            for kc in range(2):
                mv.append(P.mm(pv[:, :], ckvT[:, kc, ts_], wv[:, kc, :], start=(kc == 0), stop=(kc == 1), deps=[e2] + ld + r6))
            e5 = P.copy("act", vaug[:, t, :, 0:64], pv[:, :].rearrange("p (h d) -> p h d", d=64), deps=[mv[-1]])
            psr.readers[jv].append(e5)
            prep_w.extend([e1, e2, e3, e4, e5])
        pso = Ring(C.ps[4:6])
        kq_readers = [[], []]
        for h in range(HEADS):
            kt_, qt_ = KT[h % 2], QT[h % 2]
            hw = []
            for qc in range(NQ):
                qs = slice(qc * 512, (qc + 1) * 512)
                jk, pk, r1 = psr.next()
                mk = []
                for kc in range(2):
                    mk.append(P.mm(pk[0:64, :], wk[:, kc, h * 64:(h + 1) * 64], ckvT[:, kc, qs],
                                   start=(kc == 0), stop=(kc == 1), deps=prep_w + r1))
                ek = P.copy("dve", kt_[0:64, qs], pk[0:64, :], deps=[mk[-1]] + kq_readers[h % 2])
                psr.readers[jk].append(ek)
                jq, pq, r2 = psr.next()
                jr, pr, r3 = psr.next()
                mq = []
                mr = []
                for kc in range(3):
                    mq.append(P.mm(pq[0:96, :], wq[:, kc, h * 96:(h + 1) * 96], cqT[:, kc, qs],
                                   start=(kc == 0), stop=(kc == 2), deps=prep_w + r2))
                for kc in range(3):
                    mr.append(P.mm(pr[0:96, :], wqr[:, kc, h * 96:(h + 1) * 96], cqT[:, kc, qs],
                                   start=(kc == 0), stop=(kc == 2), deps=prep_w + r3))
                jt, rt, r4 = rt_r.next()
                lt = P.dma("sp", rt[64:96, :, :], ropeT[:, 64:96, qs].rearrange("t p s -> p t s"), deps=r4, sem=rt_r.sem(jt))
                eq = P.copy("act", qt_[0:64, qs], pq[0:64, :], deps=[mq[-1]] + kq_readers[h % 2])
                f1 = P.tt("dve", rtmp[64:96, 0, :], pq[64:96, :], rt[64:96, 0, :], ALU.mult, deps=[mq[-1], lt])
                f2 = P.tt("dve", rtmp[64:96, 1, :], pr[64:96, :], rt[64:96, 1, :], ALU.mult, deps=[mr[-1], lt, f1])
                f3 = P.tt("dve", qt_[64:96, qs], rtmp[64:96, 0, :], rtmp[64:96, 1, :], ALU.add,
                          deps=[f2] + kq_readers[h % 2])
                rt_r.readers[jt].extend([f1, f2])
                psr.readers[jq].extend([eq, f1])
                psr.readers[jr].append(f2)
                hw.extend([ek, eq, f3])
            kq_readers[h % 2] = []
            for qc in range(NQ):
                qs = slice(qc * 512, (qc + 1) * 512)
                jo, po, r1 = pso.next()
                last = None
                LA = 2
                pend = {}

                def issue_scores(kt):
                    js_, ps_s, r2 = psr.next()
                    m1 = P.mm(ps_s[:, :], kt_[0:96, kt * 128:(kt + 1) * 128], qt_[0:96, qs], deps=hw + prep_w + r2)
                    kq_readers[h % 2].append(m1)
                    pend[kt] = (js_, ps_s, m1)
                for kt in range(min(LA, NT)):
                    issue_scores(kt)
                for kt in range(NT):
                    js_, ps_s, m1 = pend.pop(kt)
                    jp_, pT, r3 = pT_r.next()
                    ex = P.act(pT[:], ps_s[:, :], AF.Exp, scale=scale, deps=[m1] + r3)
                    psr.readers[js_].append(ex)
                    if kt + LA < NT:
                        issue_scores(kt + LA)
                    m2 = P.mm(po[0:65, :], vaug[:, kt, h, :], pT[:], start=(kt == 0), stop=(kt == NT - 1),
                              deps=[ex] + r1)
                    pT_r.readers[jp_].append(m2)
                    last = m2
                g1 = P.op("dve", lambda e, o=rs, i=po: e.reciprocal(out=o[64:65, :], in_=i[64:65, :]), [last])
                jb, pb, r4 = psr.next()
                g2 = P.mm(pb[0:64, :], C.ones_f[64:65, 0:64], rs[64:65, :], deps=[g1] + r4)
                g3 = P.copy("act", bsb[:], pb[0:64, :], deps=[g2])
                psr.readers[jb].append(g3)
                jo2, ob, r5 = o_r.next()
                g4 = P.tt("dve", ob[:], po[0:64, :], bsb[:], ALU.mult, deps=[g3, last] + r5)
                pso.readers[jo].extend([g1, g4])
                s = P.dma("pool", fm_o[h * 64:(h + 1) * 64, qs], ob[:], deps=[g4], sem=o_r.sem(jo2))
                o_r.readers[jo2].append(s)
        P.run()


def phase_ssd(nc, P, C, S, fm_xa, tm_dt, tm_z, dt_bias, a_log, d_skip, norm_g, fm_ys):
    NT = S // 128
    with contextlib.ExitStack() as st:
        sb = lambda name, shape, dt: st.enter_context(nc.sbuf_tensor(name + "_L%d" % _LAYER[0], shape, dt))
        dtb = sb("p4_dtb", [128, 32], F32)
        At = sb("p4_A", [128, 32], F32)
        dsk16 = sb("p4_dsk16", [128, 16], F32)
        dsk = sb("p4_dsk", [128, 16, 64], F32)
        gt = sb("p4_gt", [128, 1024], F32)
        eps = sb("p4_eps", [128, 1], F32)
        Hin_b = sb("p4_Hinb", [128, NT, 512], BF16)
        Hst = [sb("p4_H%d" % d, [128, 2, 256], F32) for d in range(2)]
        Hbf = sb("p4_Hbf", [128, 2, 256], BF16)
        fmx_r = Ring([sb("p4_fmx%d" % i, [128, 12, 128], BF16) for i in range(2)], 2)
        dt_r = Ring([sb("p4_dt%d" % i, [128, 32], F32) for i in range(2)], 4)
        z_r = Ring([sb("p4_z%d" % i, [128, 1024], F32) for i in range(2)], 6)
        a_r = Ring([sb("p4_a%d" % i, [128, 32], F32) for i in range(2)])
        dtv_r = Ring([sb("p4_dtv%d" % i, [128, 32], F32) for i in range(2)])
        edec_r = Ring([sb("p4_edec%d" % i, [128, 96], F32) for i in range(2)])
        ws_r = Ring([sb("p4_ws%d" % i, [128, 32], F32) for i in range(2)])
        xs_r = Ring([sb("p4_xs%d" % i, [128, 1024], BF16) for i in range(2)])
        bt_r = Ring([sb("p4_bt%d" % i, [128, 256], BF16) for i in range(2)])
        xt_r = Ring([sb("p4_xt%d" % i, [128, 2, 1024], BF16) for i in range(2)])
        xd_r = Ring([sb("p4_xd%d" % i, [128, 2, 1024], BF16) for i in range(2)])
        gm_r = Ring([sb("p4_gm%d" % i, [128, 2, 4, 128], F32) for i in range(2)])
        rseg_r = Ring([sb("p4_rseg%d" % i, [128, 2, 4, 128], BF16) for i in range(3)])
        ahl_r = Ring([sb("p4_ahl%d" % i, [128, 2, 32], F32) for i in range(2)])
        ahb_r = Ring([sb("p4_ahb%d" % i, [128, 32], BF16) for i in range(2)])
        E_r = Ring([sb("p4_E%d" % i, [128, 4, 128], F32) for i in range(2)])
        MT_r = Ring([sb("p4_MT%d" % i, [128, 4, 128], BF16) for i in range(3)])
        E2_r = Ring([sb("p4_E2%d" % i, [128, 4, 128], F32) for i in range(2)])
        CTd_rp = [Ring([sb("p4_CTd%d_%d" % (par, i), [128, 4, 128], BF16) for i in range(2)]) for par in range(2)]
        CTz_r = Ring([sb("p4_CTz%d" % i, [128, 4, 128], BF16) for i in range(2)])
        y_r = Ring([sb("p4_y%d" % i, [128, 1024], F32) for i in range(2)])
        yb_r = Ring([sb("p4_yb%d" % i, [128, 1024], BF16) for i in range(2)])
        yT_r = Ring([sb("p4_yT%d" % i, [128, 8, 128], BF16) for i in range(2)], 8)
        ssq_r = Ring([sb("p4_ssq%d" % i, [128, 4], F32) for i in range(2)])
        junk = sb("p4_junk", [128, 256], F32)

        ld = [P.dma("sp", dtb[:], dt_bias.partition_broadcast(128), sem=0),
              P.dma("sp", At[:], a_log.partition_broadcast(128), sem=0),
              P.dma("sp", dsk16[:], d_skip.partition_broadcast(128), sem=0),
              P.dma("sp", gt[:], norm_g.partition_broadcast(128), sem=0)]
        i1 = P.act(At[:], At[:], AF.Exp, deps=ld)
        i2 = P.ts("dve", At[:], At[:], -1.0, None, op0=ALU.mult, deps=[i1])
        i3 = P.copy("dve", dsk[:], dsk16[:].unsqueeze(2).to_broadcast([128, 16, 64]), deps=ld)
        i4 = P.memset("pool", eps[:], 1e-6)
        i5 = P.memset("pool", Hst[0][:], 0.0)
        i6 = P.memset("pool", Hst[1][:], 0.0)
        init = ld + [i2, i3, i4, i5, i6]
        for par in range(2):
            for b in CTd_rp[par].bufs:
                init.append(P.memset("pool", b[:], 0.0))
        for b in CTz_r.bufs:
            init.append(P.memset("pool", b[:], 0.0))

        ps_small, ps_G, ps_tr, ps_st = C.ps[0], C.ps[1], C.ps[2], C.ps[3]
        ps_seg = Ring(C.ps[4:6])
        ps_y = [C.ps[6], C.ps[7]]
        rd = {"small": [], "G": [], "tr": [], "st": [], "y": []}
        fmxv = fm_xa.rearrange("(c p) s -> p c s", p=128)
        fysv = fm_ys.rearrange("(c p) s -> p c s", p=128)

        def prep(c, full):
            cs = slice(c * 128, (c + 1) * 128)
            o = {}
            jf, fmx, r = fmx_r.next()
            l1 = P.dma("sp", fmx[:], fmxv[:, :, cs], deps=r, sem=fmx_r.sem(jf))
            jd, dtt, r = dt_r.next()
            l2 = P.dma("sp", dtt[:], tm_dt[cs, :], deps=r, sem=dt_r.sem(jd))
            jv, dtv, r = dtv_r.next()
            q1 = P.tt("dve", dtv[:], dtt[:], dtb[:], ALU.add, deps=[l2] + init + r)
            dt_r.readers[jd].append(q1)
            q2 = P.act(dtv[:], dtv[:], AF.Exp, deps=[q1])
            q3 = P.act(dtv[:], dtv[:], AF.Ln, bias=1.0, deps=[q2])
            ja, a, r = a_r.next()
            q4 = P.tt("dve", a[:], dtv[:], At[:], ALU.mult, deps=[q3] + r)
            jhb, ahb, r = ahb_r.next()
            h1 = P.copy("dve", ahb[:], a[:], deps=[q4] + r)
            jhl, ahl, r = ahl_r.next()
            h2 = P.copy("dve", ahl[:, 0, :], ahb[:], deps=[h1] + r)
            h3 = P.tt("dve", ahl[:, 1, :], a[:], ahl[:, 0, :], ALU.subtract, deps=[h2])
            ahb_r.readers[jhb].append(h2)
            m = [P.mm(ps_small[:, 0:16], C.m_gt[:], a[:, 0:16], start=True, stop=False, deps=[q4] + rd["small"]),
                 P.mm(ps_small[:, 16:32], C.m_lt[:], a[:, 16:32], start=False, stop=False, deps=[q4]),
                 P.mm(ps_small[:, 32:48], C.m_le[:], a[:, 0:16], start=False, stop=False, deps=[q4]),
                 P.mm(ps_small[:, 48:64], C.m_ge[:], a[:, 16:32], start=False, stop=False, deps=[q4]),
                 P.mm(ps_small[:, 64:96], C.ones_f[:], a[:, 0:32], start=False, stop=True, deps=[q4])]
            je, edec, r = edec_r.next()
            q5 = P.act(edec[:], ps_small[:, 0:96], AF.Exp, deps=[m[-1]] + r)
            rd["small"] = [q5]
            jw, ws, r = ws_r.next()
            q6 = P.tt("dve", ws[:], dtv[:], edec[:, 0:32], ALU.mult, deps=[q5] + r)
            ptb = ps_tr[:].bitcast(BF16)
            jx, xs, r = xs_r.next()
            trs = [P.tr(ptb[:, q * 128:(q + 1) * 128], fmx[:, q, :], C.ident_b[:], deps=[l1] + rd["tr"]) for q in range(8)]
            q7 = P.copy("act", xs[:], ptb[:, :], deps=trs + r)
            jb, bt, r = bt_r.next()
            trs2 = [P.tr(ptb[:, q * 128:(q + 1) * 128], fmx[:, 8 + q, :], C.ident_b[:], deps=[l1, q7]) for q in range(2)]
            q8 = P.copy("act", bt[:], ptb[:, 0:256], deps=trs2 + r)
            rd["tr"] = [q8]
            xs3 = xs[:].rearrange("p (h d) -> p h d", d=64)
            jt, xt, r = xt_r.next()
            jq, xd, r2 = xd_r.next()
            xw = []
            for d in range(2):
                xw.append(P.tt("dve", xd[:, d, :].rearrange("p (h d) -> p h d", d=64), xs3,
                               ws[:, d * 16:(d + 1) * 16].unsqueeze(2).to_broadcast([128, 16, 64]), ALU.mult,
                               deps=[q6, q7] + r2))
                if full:
                    xw.append(P.tt("dve", xt[:, d, :].rearrange("p (h d) -> p h d", d=64), xs3,
                                   dtv[:, d * 16:(d + 1) * 16].unsqueeze(2).to_broadcast([128, 16, 64]), ALU.mult,
                                   deps=[q3, q7] + r))
            o.update(ahl=ahl, jhl=jhl, h3=h3)
            o.update(fmx=fmx, jf=jf, a=a, ja=ja, edec=edec, je=je, xs=xs, jx=jx, bt=bt, jb=jb, xt=xt, jt=jt,
                     xd=xd, jq=jq, dtv=dtv, jv=jv, ws=ws, jw=jw, ready=[l1, q4, q5, q6, q7, q8] + xw)
            return o

        def state_update(o, d, c_store=None):
            H = Hst[d]
            ups = []
            for j in range(2):
                m = P.mm(ps_st[:, :], o["bt"][:, j * 128:(j + 1) * 128], o["xd"][:, d, j * 512:(j + 1) * 512],
                         deps=o["ready"] + rd["st"])
                u = []
                for half in range(2):
                    g = 2 * j + half
                    hs = slice(half * 64, half * 64 + 64)
                    cd = o["edec"][hs, 64 + d * 16 + g * 4:64 + d * 16 + g * 4 + 4].unsqueeze(2).to_broadcast([64, 4, 64])
                    Hv = H[hs, j, :].rearrange("p (r d) -> p r d", d=64)
                    u1 = P.tt("dve", Hv, Hv, cd, ALU.mult, deps=o["ready"] + rd["H%d" % d])
                    u2 = P.tt("dve", H[hs, j, :], H[hs, j, :], ps_st[hs, half * 256:half * 256 + 256], ALU.add,
                              deps=[u1, m])
                    u.append(u2)
                rd["st"] = u
                ups.extend(u)
            return ups

        rd["H0"] = []
        rd["H1"] = []
        hb_w = [None] * NT
        prevu = [i6]
        for c in range(NT - 1, -1, -1):
            s0 = P.copy("act", Hin_b[:, c, :], Hst[1][:].rearrange("p j f -> p (j f)"), deps=prevu)
            hb_w[c] = s0
            rd["H1"] = [s0]
            if c > 0:
                o = prep(c, False)
                prevu = state_update(o, 1)
                for k in ("fmx", "a", "edec", "xs", "bt", "xd", "dtv", "ws"):
                    pass
                fmx_r.readers[o["jf"]].extend(o["ready"])
                a_r.readers[o["ja"]].extend(o["ready"])
                edec_r.readers[o["je"]].extend(prevu)
                xs_r.readers[o["jx"]].extend(o["ready"])
                bt_r.readers[o["jb"]].extend(prevu)
                xd_r.readers[o["jq"]].extend(prevu)
                dtv_r.readers[o["jv"]].extend(o["ready"])
                ws_r.readers[o["jw"]].extend(o["ready"])
        prevu = [i5]
        hbf_rd = []
        for c in range(NT):
            cs = slice(c * 128, (c + 1) * 128)
            o = prep(c, True)
            fmx, a, edec = o["fmx"], o["a"], o["edec"]
            rdy = o["ready"]
            hc = P.copy("act", Hbf[:].rearrange("p j f -> p (j f)"), Hst[0][:].rearrange("p j f -> p (j f)"),
                        deps=prevu + hbf_rd)
            rd["H0"] = [hc]
            hbf_rd = []
            gms = []
            jg, gm, r = gm_r.next()
            jz, CTz, rz_ = CTz_r.next()
            czw = []
            for g in range(4):
                hs = slice((g % 2) * 64, (g % 2) * 64 + 64)
                czw.append(P.copy("pool", CTz[hs, g, :], fmx[hs, 10 + g // 2, :], deps=rdy + init + rz_))
            for g in range(4):
                gms.append(P.mm(ps_G[:, g * 128:(g + 1) * 128], fmx[:, 8 + g // 2, :], CTz[:, g, :],
                                start=(g == 0), stop=(g == 3), deps=rdy + czw + rd["G"]))
            CTz_r.readers[jz].extend(gms)
            ge = []
            for g in range(4):
                ge.append(P.tt("dve", gm[:, 0, g, :], ps_G[:, g * 128:(g + 1) * 128], C.m_le[:], ALU.mult, deps=gms + r))
                ge.append(P.tt("dve", gm[:, 1, g, :], ps_G[:, g * 128:(g + 1) * 128], C.m_ge[:], ALU.mult, deps=gms + r))
            rd["G"] = ge
            first_y = [True, True]
            ymm = []

            def stage_a(d, g):
                lhs_seg = C.m_gt if d == 0 else C.m_lt
                mask2 = C.m_le if d == 0 else C.m_ge
                lhs_seg = C.m_gt_b if d == 0 else C.m_lt_b
                jr, rseg, r = rseg_r.next()
                rs_w = []
                for rr in range(4):
                    hcol = d * 16 + g * 4 + rr
                    for hl in range(2):
                        rs_w.append(P.ts("dve", rseg[:, hl, rr, :], mask2[:], o["ahl"][:, hl, hcol:hcol + 1], None,
                                         op0=ALU.mult, deps=rdy + [o["h3"]] + r))
                jp, pseg, r2 = ps_seg.next()
                P.mm(pseg[:, :], lhs_seg[:], rseg[:, 0].rearrange("p r l -> p (r l)"), start=True, stop=False,
                     deps=rs_w + r2)
                m1 = P.mm(pseg[:, :], lhs_seg[:], rseg[:, 1].rearrange("p r l -> p (r l)"), start=False, stop=True,
                          deps=rs_w)
                jE, E, r3 = E_r.next()
                x1 = P.act(E[:].rearrange("p r l -> p (r l)"), pseg[:, :], AF.Exp, deps=[m1] + r3)
                ps_seg.readers[jp].append(x1)
                jp2, pseg2, r5 = ps_seg.next()
                P.mm(pseg2[:, :], C.ones_b[:], rseg[:, 0].rearrange("p r l -> p (r l)"), start=True, stop=False,
                     deps=rs_w + r5)
                m2 = P.mm(pseg2[:, :], C.ones_b[:], rseg[:, 1].rearrange("p r l -> p (r l)"), start=False, stop=True,
                          deps=rs_w)
                rseg_r.readers[jr].extend([m1, m2])
                hs = slice((g % 2) * 64, (g % 2) * 64 + 64)
                jE2, E2, r6 = E2_r.next()
                x3 = P.act(E2[hs].rearrange("p r l -> p (r l)"), pseg2[hs, :], AF.Exp, deps=[m2] + r6)
                ps_seg.readers[jp2].append(x3)
                return dict(jE=jE, E=E, x1=x1, jE2=jE2, E2=E2, x3=x3)

            def stage_b(d, g, A):
                hs = slice((g % 2) * 64, (g % 2) * 64 + 64)
                jM, MT, r4 = MT_r.next()
                x2 = P.tt("dve", MT[:], A["E"][:], gm[:, d, g, :].unsqueeze(1).to_broadcast([128, 4, 128]), ALU.mult,
                          deps=[A["x1"]] + ge + r4)
                E_r.readers[A["jE"]].append(x2)
                CTd_r = CTd_rp[g % 2]
                jC, CTd, r7 = CTd_r.next()
                x4 = P.tt("dve", CTd[hs], A["E2"][hs], fmx[hs, 10 + g // 2, :].unsqueeze(1).to_broadcast([64, 4, 128]),
                          ALU.mult, deps=[A["x3"]] + r7 + init)
                E2_r.readers[A["jE2"]].append(x4)
                if d == 0:
                    hsrc = Hbf[:, g // 2, :]
                    hdeps = [hc]
                else:
                    hsrc = Hin_b[:, c, (g // 2) * 256:(g // 2) * 256 + 256]
                    hdeps = [hb_w[c]]
                for rr in range(4):
                    h = g * 4 + rr
                    bank = ps_y[h // 8]
                    col = (h % 8) * 64
                    ya = P.mm(bank[:, col:col + 64], MT[:, rr, :], o["xt"][:, d, h * 64:(h + 1) * 64],
                              start=first_y[h // 8], stop=False, deps=[x2] + rdy + rd["y"])
                    first_y[h // 8] = False
                    last = (d == 1 and g == 3 and rr == 3) or (d == 1 and g == 1 and rr == 3)
                    yo = P.mm(bank[:, col:col + 64], CTd[:, rr, :], hsrc[:, rr * 64:(rr + 1) * 64],
                              start=False, stop=last, deps=[x4] + hdeps)
                    ymm.extend([ya, yo])
                    MT_r.readers[jM].append(ya)
                    CTd_r.readers[jC].append(yo)
                    if d == 0:
                        hbf_rd.append(yo)

            its = [(d, g) for d in range(2) for g in range(4)]
            Acur = stage_a(*its[0])
            for ii, (d, g) in enumerate(its):
                Anext = stage_a(*its[ii + 1]) if ii + 1 < len(its) else None
                stage_b(d, g, Acur)
                Acur = Anext
            gm_r.readers[jg].extend(ymm)
            prevu = state_update(o, 0)
            jy, y, r = y_r.next()
            jz, z, rz = z_r.next()
            lz = P.dma("sp", z[:], tm_z[cs, :], deps=rz, sem=z_r.sem(jz))
            e1 = P.tt("dve", y[:].rearrange("p (h d) -> p h d", d=64), o["xs"][:].rearrange("p (h d) -> p h d", d=64),
                      dsk[:], ALU.mult, deps=rdy + init + r)
            e2 = P.tt("dve", y[:, 0:512], ps_y[0][:, :], y[:, 0:512], ALU.add, deps=[e1] + ymm)
            e3 = P.tt("dve", y[:, 512:1024], ps_y[1][:, :], y[:, 512:1024], ALU.add, deps=[e1] + ymm)
            rd["y"] = [e2, e3]
            e4 = P.tt("dve", y[:], y[:], z[:], ALU.mult, deps=[e2, e3, lz])
            z_r.readers[jz].append(e4)
            js, ssq, r = ssq_r.next()
            sq = []
            for g in range(4):
                sq.append(P.act(junk[:], y[:, g * 256:(g + 1) * 256], AF.Square, accum_out=ssq[:, g:g + 1],
                                deps=[e4] + r + sq[-1:]))
            f1 = P.act(ssq[:], ssq[:], AF.Sqrt, bias=eps[:, 0:1], scale=1.0 / 256.0, deps=sq)
            f2 = P.op("dve", lambda e, o_=ssq: e.reciprocal(out=o_[:], in_=o_[:]), [f1])
            jyb, yb, r = yb_r.next()
            f3 = []
            for g in range(4):
                f3.append(P.stt(yb[:, g * 256:(g + 1) * 256], y[:, g * 256:(g + 1) * 256], ssq[:, g:g + 1],
                                gt[:, g * 256:(g + 1) * 256], ALU.mult, ALU.mult, deps=[f2] + r))
            y_r.readers[jy].extend(f3)
            ssq_r.readers[js].extend(f3)
            ptb = ps_tr[:].bitcast(BF16)
            trs = [P.tr(ptb[:, q * 128:(q + 1) * 128], yb[:, q * 128:(q + 1) * 128], C.ident_b[:], deps=f3 + rd["tr"])
                   for q in range(8)]
            yb_r.readers[jyb].extend(trs)
            jT, yT, r = yT_r.next()
            f4 = P.copy("act", yT[:].rearrange("p q t -> p (q t)"), ptb[:, :], deps=trs + r)
            rd["tr"] = [f4]
            s = P.dma("pool", fysv[:, :, cs], yT[:], deps=[f4], sem=yT_r.sem(jT))
            yT_r.readers[jT].append(s)
            allr = ymm + prevu + [e1]
            fmx_r.readers[o["jf"]].extend(allr)
            a_r.readers[o["ja"]].extend(allr)
            edec_r.readers[o["je"]].extend(allr)
            xs_r.readers[o["jx"]].extend(allr)
            bt_r.readers[o["jb"]].extend(allr)
            xd_r.readers[o["jq"]].extend(allr)
            xt_r.readers[o["jt"]].extend(allr)
            dtv_r.readers[o["jv"]].extend(allr)
            ws_r.readers[o["jw"]].extend(allr)
            ahl_r.readers[o["jhl"]].extend(allr)
        P.run()


CAP = 640
NSLOT = NE * CAP


def layernorm_tile(P, v, stats, mv, g_t, b_t, eps_t, deps):
    s1 = P.op("dve", lambda e: e.bn_stats(out=stats[:, 0:6], in_=v[:, 0:512]), deps)
    s2 = P.op("dve", lambda e: e.bn_stats(out=stats[:, 6:12], in_=v[:, 512:1024]), deps)
    s3 = P.op("dve", lambda e: e.bn_aggr(out=mv[:, 0:2], in_=stats[:, 0:12]), [s1, s2])
    s4 = P.act(mv[:, 1:2], mv[:, 1:2], AF.Sqrt, bias=eps_t, deps=[s3])
    s5 = P.op("dve", lambda e: e.reciprocal(out=mv[:, 1:2], in_=mv[:, 1:2]), [s4])
    s6 = P.ts("dve", v, v, mv[:, 0:1], mv[:, 1:2], op0=ALU.subtract, op1=ALU.mult, deps=[s5])
    s7 = P.tt("dve", v, v, g_t, ALU.mult, deps=[s6])
    return P.tt("dve", v, v, b_t, ALU.add, deps=[s7])


def phase_merge(nc, P, C, S, x, fm_o, fm_ys, fm_cv, fm_g, w_ba, w_bs, w_bc, b_bc, w_out, ln_g, ln_b,
                router_w, router_b, x1, Xg, tm_slot, tm_gate):
    NQ = S // 512
    with contextlib.ExitStack() as st:
        sb = lambda name, shape, dt: st.enter_context(nc.sbuf_tensor(name + "_L%d" % _LAYER[0], shape, dt))
        wa = sb("p5_wa", [128, 4, D], BF16)
        ws_ = sb("p5_ws", [128, 8, D], BF16)
        wc = sb("p5_wc", [128, 4, D], BF16)
        wo = sb("p5_wo", [128, 8, D], BF16)
        wr = sb("p5_wr", [128, 8, NE], F32)
        bc = sb("p5_bc", [128, 8], F32)
        lg = sb("p5_lg", [128, D], F32)
        lb = sb("p5_lb", [128, D], F32)
        rb = sb("p5_rb", [128, NE], F32)
        eps = sb("p5_eps", [128, 1], F32)
        eiota = sb("p5_eiota", [128, NE], F32)
        cum = sb("p5_cum", [128, NE], F32)
        fo_r = Ring([sb("p5_fo%d" % i, [128, 4, 512], BF16) for i in range(1)], 2)
        fy_r = Ring([sb("p5_fy%d" % i, [128, 8, 512], BF16) for i in range(1)], 4)
        fc_r = Ring([sb("p5_fc%d" % i, [128, 4, 512], BF16) for i in range(1)], 6)
        fg_r = Ring([sb("p5_fg%d" % i, [128, 24, 512], BF16) for i in range(1)], 8)
        mix_r = Ring([sb("p5_mix%d" % i, [128, 8, 512], BF16) for i in range(2)])
        t_r = Ring([sb("p5_t%d" % i, [128, 512], F32) for i in range(3)])
        xr_r = Ring([sb("p5_xr%d" % i, [128, D], F32) for i in range(2)], 10)
        v_r = Ring([sb("p5_v%d" % i, [128, D], F32) for i in range(2)], 12)
        vb_r = Ring([sb("p5_vb%d" % i, [128, D], BF16) for i in range(2)], 14)
        xT_r = Ring([sb("p5_xT%d" % i, [128, 8, 128], F32) for i in range(2)])
        sm_r = Ring([sb("p5_sm%d" % i, [128, 480], F32) for i in range(2)], 16)
        sl_r = Ring([sb("p5_sl%d" % i, [128, 4], I32) for i in range(2)], 18)
        stats = sb("p5_stats", [128, 12], F32)
        mv = sb("p5_mv", [128, 2], F32)

        ld = []
        for kc in range(4):
            ld.append(P.dma("pool", wa[:, kc, :], w_ba[kc * 128:(kc + 1) * 128, :], sem=0))
            ld.append(P.dma("pool", wc[:, kc, :], w_bc[kc * 128:(kc + 1) * 128, :], sem=0))
        for kc in range(8):
            ld.append(P.dma("pool", ws_[:, kc, :], w_bs[kc * 128:(kc + 1) * 128, :], sem=0))
            ld.append(P.dma("pool", wo[:, kc, :], w_out[kc * 128:(kc + 1) * 128, :], sem=0))
            ld.append(P.dma("sp", wr[:, kc, :], router_w[kc * 128:(kc + 1) * 128, :], sem=1))
            ld.append(P.dma("sp", bc[:, kc:kc + 1], b_bc[kc * 128:(kc + 1) * 128].rearrange("(p o) -> p o", o=1), sem=1))
        ld.append(P.dma("sp", lg[:], ln_g.partition_broadcast(128), sem=1))
        ld.append(P.dma("sp", lb[:], ln_b.partition_broadcast(128), sem=1))
        ld.append(P.dma("sp", rb[:], router_b.partition_broadcast(128), sem=1))
        ld.append(P.memset("pool", eps[:], 1e-5))
        ld.append(P.memset("pool", cum[:], 0.0))
        ld.append(P.op("pool", lambda e: e.iota(eiota[:], pattern=[[CAP, NE]], base=0, channel_multiplier=0,
                                                allow_small_or_imprecise_dtypes=True)))
        zf = []
        fov = fm_o.rearrange("(c p) s -> p c s", p=128)
        fyv = fm_ys.rearrange("(c p) s -> p c s", p=128)
        fcv = fm_cv.rearrange("(c p) s -> p c s", p=128)
        fgv = fm_g.rearrange("(c p) s -> p c s", p=128)
        psr = Ring(C.ps[0:4])
        pso = Ring(C.ps[4:6])
        ps_t = C.ps[6]
        ps_r = C.ps[7]
        rd_t = []
        rd_r = []
        cum_w = []
        for qc in range(NQ):
            qs = slice(qc * 512, (qc + 1) * 512)
            jo, fo, r = fo_r.next()
            l1 = P.dma("sp", fo[:], fov[:, :, qs], deps=r, sem=fo_r.sem(jo))
            jy, fy, r = fy_r.next()
            l2 = P.dma("sp", fy[:], fyv[:, :, qs], deps=r, sem=fy_r.sem(jy))
            jc, fc, r = fc_r.next()
            l3 = P.dma("sp", fc[:], fcv[:, :, qs], deps=r, sem=fc_r.sem(jc))
            jg, fg, r = fg_r.next()
            l4 = P.dma("sp", fg[:], fgv[:, :, qs], deps=r, sem=fg_r.sem(jg))
            jm, mix, rmix = mix_r.next()
            lds = [l1, l2, l3, l4] + ld
            mixw = []
            allmm = []
            for f in range(8):
                fs = slice(f * 128, (f + 1) * 128)
                ja, pa, r = psr.next()
                ma = [P.mm(pa[:, :], wa[:, kc, fs], fo[:, kc, :], start=(kc == 0), stop=(kc == 3), deps=lds + r)
                      for kc in range(4)]
                jt1, t1, r = t_r.next()
                e1 = P.tt("dve", t1[:], pa[:, :], fg[:, f, :], ALU.mult, deps=[ma[-1]] + r)
                psr.readers[ja].append(e1)
                js, pss, r = psr.next()
                ms = [P.mm(pss[:, :], ws_[:, kc, fs], fy[:, kc, :], start=(kc == 0), stop=(kc == 7), deps=lds + r)
                      for kc in range(8)]
                jt2, t2, r = t_r.next()
                e2 = P.tt("dve", t2[:], pss[:, :], fg[:, 8 + f, :], ALU.mult, deps=[ms[-1]] + r)
                psr.readers[js].append(e2)
                e3 = P.tt("dve", t1[:], t1[:], t2[:], ALU.add, deps=[e1, e2])
                t_r.readers[jt2].append(e3)
                jc2, pc, r = psr.next()
                mc = [P.mm(pc[:, :], wc[:, kc, fs], fc[:, kc, :], start=(kc == 0), stop=(kc == 3), deps=lds + r)
                      for kc in range(4)]
                jt3, t3, r = t_r.next()
                e4 = P.stt(t3[:], pc[:, :], bc[:, f:f + 1], fg[:, 16 + f, :], ALU.add, ALU.mult, deps=[mc[-1]] + r)
                psr.readers[jc2].append(e4)
                e5 = P.tt("dve", mix[:, f, :], t1[:], t3[:], ALU.add, deps=[e3, e4] + rmix)
                t_r.readers[jt1].append(e5)
                t_r.readers[jt3].append(e5)
                mixw.append(e5)
                allmm.extend([ma[-1], ms[-1], mc[-1]])
            fo_r.readers[jo].extend(allmm)
            fy_r.readers[jy].extend(allmm)
            fc_r.readers[jc].extend(allmm)
            fg_r.readers[jg].extend(mixw)
            for tt in range(4):
                t0 = qc * 512 + tt * 128
                tsl = slice(t0, t0 + 128)
                jx, xr, r = xr_r.next()
                lx = P.dma("sp", xr[:], x[tsl, :], deps=r, sem=xr_r.sem(jx))
                jv, v, rv = v_r.next()
                ev = []
                for half in range(2):
                    jp, po, r = pso.next()
                    mo = [P.mm(po[:, :], mix[:, f, tt * 128:(tt + 1) * 128], wo[:, f, half * 512:(half + 1) * 512],
                               start=(f == 0), stop=(f == 7), deps=mixw + ld + r) for f in range(8)]
                    e = P.stt(v[:, half * 512:(half + 1) * 512], xr[:, half * 512:(half + 1) * 512], float(DN_ALPHA),
                              po[:, :], ALU.mult, ALU.add, deps=[mo[-1], lx] + rv)
                    pso.readers[jp].append(e)
                    mix_r.readers[jm].append(mo[-1])
                    ev.append(e)
                xr_r.readers[jx].extend(ev)
                lnl = layernorm_tile(P, v[:], stats, mv, lg[:], lb[:], eps[:, 0:1], ev + ld)
                s1 = P.dma("pool", x1[tsl, :], v[:], deps=[lnl], sem=v_r.sem(jv))
                jb, vb, r = vb_r.next()
                cvb = P.copy("act", vb[:], v[:], deps=[lnl] + r)
                jT, xT, rT = xT_r.next()
                tw = []
                for rnd in range(2):
                    trs = [P.tr(ps_t[:, q * 128:(q + 1) * 128], v[:, (rnd * 4 + q) * 128:(rnd * 4 + q + 1) * 128],
                                C.ident_f[:], deps=[lnl] + rd_t) for q in range(4)]
                    ec = P.copy("act", xT[:, rnd * 4:rnd * 4 + 4, :].rearrange("p q t -> p (q t)"), ps_t[:, :],
                                deps=trs + rT)
                    rd_t = [ec]
                    tw.append(ec)
                v_r.readers[jv].extend([s1, cvb] + tw)
                ml = [P.mm(ps_r[:, 0:NE], xT[:, kc, :], wr[:, kc, :], start=(kc == 0), stop=(kc == 7), deps=tw + ld + rd_r)
                      for kc in range(8)]
                xT_r.readers[jT].append(ml[-1])
                jsm, sm, r = sm_r.next()
                LG, MK, EX, PO, T1, T2 = (sm[:, 0:32], sm[:, 32:64], sm[:, 64:96], sm[:, 96:128], sm[:, 128:160],
                                          sm[:, 160:192])
                V8, NV0, ZS, SLF, GK = sm[:, 192:200], sm[:, 200:201], sm[:, 201:202], sm[:, 204:208], sm[:, 208:212]
                a1 = P.tt("dve", LG, ps_r[:, 0:NE], rb[:], ALU.add, deps=[ml[-1]] + r)
                a2 = P.op("dve", lambda e, o=V8, i=LG: e.max(out=o, in_=i), [a1])
                a3 = P.ts("dve", MK, LG, V8[:, 3:4], None, op0=ALU.is_ge, deps=[a2])
                a4 = P.ts("dve", NV0, V8[:, 0:1], -1.0, None, op0=ALU.mult, deps=[a2])
                a5 = P.act(EX, LG, AF.Exp, bias=NV0, deps=[a4])
                a6 = P.tt("dve", EX, EX, MK, ALU.mult, deps=[a5, a3])
                a7 = P.op("dve", lambda e, o=ZS, i=EX: e.reduce_sum(out=o, in_=i, axis=AX.X), [a6])
                a8 = P.op("dve", lambda e, o=ZS: e.reciprocal(out=o, in_=o), [a7])
                a9 = P.ts("dve", EX, EX, ZS, None, op0=ALU.mult, deps=[a8])
                mp1 = P.mm(ps_r[:, 64:64 + NE], C.m_lt[:], MK, start=True, stop=False, deps=[a3, a1])
                mp2 = P.mm(ps_r[:, 64:64 + NE], C.ident_f[:], cum[:], start=False, stop=False, deps=cum_w + ld)
                mp3 = P.mm(ps_r[:, 128:128 + NE], C.ones_f[:], MK, start=False, stop=True, deps=[a3])
                b1 = P.ts("dve", T1, ps_r[:, 64:64 + NE], float(CAP), None, op0=ALU.is_ge, deps=[mp2, mp3])
                b2 = P.tt("dve", PO, ps_r[:, 64:64 + NE], eiota[:], ALU.add, deps=[mp2, mp3])
                b3 = P.stt(PO, T1, 1.0e6, PO, ALU.mult, ALU.add, deps=[b1, b2])
                b4 = P.tt("dve", cum[:], cum[:], ps_r[:, 128:128 + NE], ALU.add, deps=[mp3, mp2])
                cum_w = [b4]
                b5 = P.ts("dve", T1, T1, -1.0, 1.0, op0=ALU.mult, op1=ALU.add, deps=[b3])
                b6 = P.tt("dve", EX, EX, T1, ALU.mult, deps=[b5, a9])
                rd_r = [a1, b1, b2, b4]
                T4 = sm[:, 212:212 + 128].rearrange("p (k e) -> p k e", k=4)
                c1 = P.tt("dve", T4, LG.unsqueeze(1).to_broadcast([128, 4, NE]),
                          V8[:, 0:4].unsqueeze(2).to_broadcast([128, 4, NE]), ALU.is_equal, deps=[b6])
                T5 = sm[:, 340:340 + 128].rearrange("p (k e) -> p k e", k=4)
                c2 = P.tt("dve", T5, T4, PO.unsqueeze(1).to_broadcast([128, 4, NE]), ALU.mult, deps=[c1])
                c3 = P.op("dve", lambda e, o=SLF, i=T5: e.reduce_sum(out=o, in_=i, axis=AX.X), [c2])
                c4 = P.tt("dve", T5, T4, EX.unsqueeze(1).to_broadcast([128, 4, NE]), ALU.mult, deps=[c3])
                lastk = P.op("dve", lambda e, o=GK, i=T5: e.reduce_sum(out=o, in_=i, axis=AX.X), [c4])
                jsl, sl, r = sl_r.next()
                d1 = P.copy("dve", sl[:], SLF, deps=[lastk] + r)
                sc = []
                for k in range(4):
                    sc.append(P.idma(Xg[:, :], bass.IndirectOffsetOnAxis(ap=sl[:, k:k + 1], axis=0), vb[:], None,
                                     deps=[d1, cvb] + zf, sem=vb_r.sem(jb), bounds_check=NSLOT - 1, oob_is_err=False))
                vb_r.readers[jb].extend(sc)
                s2 = P.dma("pool", tm_slot[tsl, :], sl[:], deps=[d1], sem=sl_r.sem(jsl))
                s3 = P.dma("pool", tm_gate[tsl, :], GK, deps=[lastk], sem=sm_r.sem(jsm))
                sl_r.readers[jsl].extend(sc + [s2])
                sm_r.readers[jsm].extend([s3, d1])
        P.run()


def phase_experts(nc, P, C, Xg, w_gu, b_gu, w_dn, b_dn, Yg):
    NJ = CAP // 128
    with contextlib.ExitStack() as st:
        sb = lambda name, shape, dt: st.enter_context(nc.sbuf_tensor(name + "_L%d" % _LAYER[0], shape, dt))
        wg_r = Ring([sb("p7_wg%d" % i, [128, 8, 2 * DFF], BF16) for i in range(2)], 0)
        wd_r = Ring([sb("p7_wd%d" % i, [128, 8, D], BF16) for i in range(2)], 2)
        bg_r = Ring([sb("p7_bg%d" % i, [128, 16], F32) for i in range(2)], 4)
        bd_r = Ring([sb("p7_bd%d" % i, [128, D], F32) for i in range(2)], 6)
        xs_r = Ring([sb("p7_xs%d" % i, [128, D], BF16) for i in range(3)], 8)
        xT_r = Ring([sb("p7_xT%d" % i, [128, 8, CAP], BF16) for i in range(2)])
        aT_r = Ring([sb("p7_aT%d" % i, [128, 8, CAP], BF16) for i in range(1)])
        g_r = Ring([sb("p7_g%d" % i, [128, 512], F32) for i in range(2)])
        s_r = Ring([sb("p7_s%d" % i, [128, 512], F32) for i in range(2)])
        u_r = Ring([sb("p7_u%d" % i, [128, 512], F32) for i in range(2)])
        y_r = Ring([sb("p7_y%d" % i, [128, D], F32) for i in range(2)], 11)
        psr = Ring(C.ps[0:6])
        ptr = Ring(C.ps[6:8])
        colgroups = []
        c0 = 0
        while c0 < CAP:
            n = min(512, CAP - c0)
            colgroups.append((c0, n))
            c0 += n
        def load_expert(e):
            jw, wg, r = wg_r.next()
            lw = []
            for kc in range(8):
                lw.append(P.dma("pool", wg[:, kc, :], w_gu[e, kc * 128:(kc + 1) * 128, :], deps=r, sem=wg_r.sem(jw)))
            jd, wd, r = wd_r.next()
            for kc in range(8):
                lw.append(P.dma("pool", wd[:, kc, :], w_dn[e, kc * 128:(kc + 1) * 128, :], deps=r, sem=wd_r.sem(jd)))
            jb, bg, r = bg_r.next()
            lw.append(P.dma("sp", bg[:], b_gu[e, :].rearrange("(c p) -> p c", p=128), deps=r, sem=bg_r.sem(jb)))
            jbd, bd, r = bd_r.next()
            lw.append(P.dma("sp", bd[:], b_dn[e, :].partition_broadcast(128), deps=r, sem=bd_r.sem(jbd)))
            return (jw, wg, jd, wd, jb, bg, jbd, bd, lw)

        nxt = load_expert(0)
        for e in range(NE):
            jw, wg, jd, wd, jb, bg, jbd, bd, lw = nxt
            jT, xT, rT = xT_r.next()
            xw = []
            for j in range(NJ):
                r0 = e * CAP + j * 128
                jx, xs, r = xs_r.next()
                lx = P.dma("sp", xs[:], Xg[r0:r0 + 128, :], deps=r, sem=xs_r.sem(jx))
                for kc in range(0, 8, 4):
                    jp, pt, r3 = ptr.next()
                    ptb = pt[:].bitcast(BF16)
                    trs = [P.tr(ptb[:, q * 128:(q + 1) * 128], xs[:, (kc + q) * 128:(kc + q + 1) * 128], C.ident_b[:],
                                deps=[lx] + r3) for q in range(4)]
                    ev = P.copy("dve", xT[:, kc:kc + 4, j * 128:(j + 1) * 128],
                                ptb[:, 0:512].rearrange("p (q t) -> p q t", q=4), deps=trs + rT)
                    ptr.readers[jp].append(ev)
                    xs_r.readers[jx].extend(trs)
                    xw.append(ev)
            if e + 1 < NE:
                nxt = load_expert(e + 1)
            ja, aT, ra = aT_r.next()
            aw = []
            hmm = []
            for f in range(8):
                for (c0, n) in colgroups:
                    jg, pg, r1 = psr.next()
                    mg = [P.mm(pg[:, 0:n], wg[:, kc, f * 128:(f + 1) * 128], xT[:, kc, c0:c0 + n],
                               start=(kc == 0), stop=(kc == 7), deps=xw + lw + r1) for kc in range(8)]
                    ju, pu, r2 = psr.next()
                    mu = [P.mm(pu[:, 0:n], wg[:, kc, DFF + f * 128:DFF + (f + 1) * 128], xT[:, kc, c0:c0 + n],
                               start=(kc == 0), stop=(kc == 7), deps=xw + lw + r2) for kc in range(8)]
                    hmm.extend([mg[-1], mu[-1]])
                    jgg, gt, r3 = g_r.next()
                    h1 = P.ts("dve", gt[:, 0:n], pg[:, 0:n], bg[:, f:f + 1], LIMIT, op0=ALU.add, op1=ALU.min,
                              deps=[mg[-1]] + r3)
                    psr.readers[jg].append(h1)
                    jss, sg, r4 = s_r.next()
                    h2 = P.act(sg[:, 0:n], gt[:, 0:n], AF.Sigmoid, scale=SALPHA, deps=[h1] + r4)
                    h3 = P.tt("dve", gt[:, 0:n], gt[:, 0:n], sg[:, 0:n], ALU.mult, deps=[h2])
                    s_r.readers[jss].append(h3)
                    juu, ut, r5 = u_r.next()
                    h4 = P.ts("dve", ut[:, 0:n], pu[:, 0:n], bg[:, 8 + f:9 + f], LIMIT, op0=ALU.add, op1=ALU.min,
                              deps=[mu[-1]] + r5)
                    psr.readers[ju].append(h4)
                    h5 = P.ts("dve", ut[:, 0:n], ut[:, 0:n], -LIMIT, 1.0, op0=ALU.max, op1=ALU.add, deps=[h4])
                    h6 = P.tt("dve", aT[:, f, c0:c0 + n], ut[:, 0:n], gt[:, 0:n], ALU.mult, deps=[h5, h3] + ra)
                    g_r.readers[jgg].append(h6)
                    u_r.readers[juu].append(h6)
                    aw.append(h6)
            xT_r.readers[jT].extend(hmm)
            wg_r.readers[jw].extend(hmm)
            bg_r.readers[jb].extend(aw)
            dmm = []
            yw = []
            for j in range(NJ):
                r0 = e * CAP + j * 128
                jy, y, ry = y_r.next()
                evs = []
                for half in range(2):
                    jp, pd, r1 = psr.next()
                    md = [P.mm(pd[:, :], aT[:, f, j * 128:(j + 1) * 128], wd[:, f, half * 512:(half + 1) * 512],
                               start=(f == 0), stop=(f == 7), deps=aw + lw + r1) for f in range(8)]
                    dmm.append(md[-1])
                    ev = P.tt("dve", y[:, half * 512:(half + 1) * 512], pd[:, :], bd[:, half * 512:(half + 1) * 512],
                              ALU.add, deps=[md[-1]] + ry)
                    psr.readers[jp].append(ev)
                    evs.append(ev)
                s = P.dma("pool", Yg[r0:r0 + 128, :], y[:], deps=evs, sem=y_r.sem(jy))
                y_r.readers[jy].append(s)
                yw.extend(evs)
            aT_r.readers[ja].extend(dmm)
            wd_r.readers[jd].extend(dmm)
            bd_r.readers[jbd].extend(yw)
        P.run()


def phase_combine(nc, P, C, S, x1, Yg, tm_slot, tm_gate, ln_g, ln_b, xout):
    NT = S // 128
    with contextlib.ExitStack() as st:
        sb = lambda name, shape, dt: st.enter_context(nc.sbuf_tensor(name + "_L%d" % _LAYER[0], shape, dt))
        lg = sb("p8_lg", [128, D], F32)
        lb = sb("p8_lb", [128, D], F32)
        eps = sb("p8_eps", [128, 1], F32)
        stats = sb("p8_stats", [128, 12], F32)
        mv = sb("p8_mv", [128, 2], F32)
        x_r = Ring([sb("p8_x%d" % i, [128, D], F32) for i in range(3)], 2)
        sl_r = Ring([sb("p8_sl%d" % i, [128, 4], I32) for i in range(3)], 5)
        gk_r = Ring([sb("p8_gk%d" % i, [128, 4], F32) for i in range(3)], 8)
        yk_r = Ring([sb("p8_yk%d" % i, [128, D], F32) for i in range(12)], 8)
        ld = [P.dma("sp", lg[:], ln_g.partition_broadcast(128), sem=0),
              P.dma("sp", lb[:], ln_b.partition_broadcast(128), sem=0),
              P.memset("pool", eps[:], 1e-5)]
        for b in yk_r.bufs:
            ld.append(P.memset("pool", b[:], 0.0))
        for t in range(NT):
            tsl = slice(t * 128, (t + 1) * 128)
            jx, xt, r = x_r.next()
            l1 = P.dma("sp", xt[:], x1[tsl, :], deps=r, sem=x_r.sem(jx))
            js, sl, r = sl_r.next()
            l2 = P.dma("sp", sl[:], tm_slot[tsl, :], deps=r, sem=sl_r.sem(js))
            jg, gk, r = gk_r.next()
            l3 = P.dma("sp", gk[:], tm_gate[tsl, :], deps=r, sem=gk_r.sem(jg))
            prev = P.ts("dve", xt[:], xt[:], float(DN_ALPHA), None, op0=ALU.mult, deps=[l1])
            gs = []
            for k in range(4):
                jy, yk, r = yk_r.next()
                g = P.idma(yk[:], None, Yg[:, :], bass.IndirectOffsetOnAxis(ap=sl[:, k:k + 1], axis=0),
                           deps=[l2] + r + ld, sem=yk_r.sem(jy), bounds_check=NSLOT - 1, oob_is_err=False)
                prev = P.stt(xt[:], yk[:], gk[:, k:k + 1], xt[:], ALU.mult, ALU.add, deps=[g, l3, prev])
                yk_r.readers[jy].append(prev)
                gs.append(g)
            sl_r.readers[js].extend(gs)
            gk_r.readers[jg].append(prev)
            lnl = layernorm_tile(P, xt[:], stats, mv, lg[:], lb[:], eps[:, 0:1], [prev] + ld)
            s = P.dma("act", xout[tsl, :], xt[:], deps=[lnl], sem=11 + jx)
            x_r.readers[jx].append(s)
        P.run()


PARAM_SHAPES = [
    ("w_in", [DEPTH, D, NIN]), ("b_in", [DEPTH, NIN]), ("mla_q_norm", [DEPTH, QL]), ("mla_kv_norm", [DEPTH, KVL]),
    ("mla_w_uq", [DEPTH, QL, HEADS * (DN + DR)]), ("mla_w_ukv", [DEPTH, KVL, HEADS * (DN + DV)]),
    ("w_br_attn", [DEPTH, HEADS * DV, D]), ("ssm_conv_w", [DEPTH, SCONV, CDIM]), ("ssm_conv_b", [DEPTH, CDIM]),
    ("ssm_dt_bias", [DEPTH, 32]), ("ssm_a_log", [DEPTH, 32]), ("ssm_d", [DEPTH, SH]), ("ssm_norm", [DEPTH, DI]),
    ("w_br_ssm", [DEPTH, DI, D]), ("cnv_dw_w", [DEPTH, CW, CCH]), ("cnv_dw_b", [DEPTH, CCH]),
    ("cnv_ln_g", [DEPTH, CCH]), ("cnv_ln_b", [DEPTH, CCH]), ("w_br_conv", [DEPTH, CCH, D]), ("b_br_conv", [DEPTH, D]),
    ("w_out", [DEPTH, D, D]), ("ln1_g", [DEPTH, D]), ("ln1_b", [DEPTH, D]), ("router_w", [DEPTH, D, NE]),
    ("router_b", [DEPTH, NE]), ("moe_w_gate_up", [DEPTH, NE, D, 2 * DFF]), ("moe_b_gate_up", [DEPTH, NE, 2 * DFF]),
    ("moe_w_down", [DEPTH, NE, DFF, D]), ("moe_b_down", [DEPTH, NE, D]), ("ln2_g", [DEPTH, D]), ("ln2_b", [DEPTH, D]),
]


def build_program(S, depth=DEPTH, debug=False, stop_after=99, skip=()):
    nc = bass.Bass("TRN2", target_bir_lowering=False)
    x = nc.dram_tensor("x", [S, D], F32, kind="ExternalInput").ap()
    W = {n: nc.dram_tensor(n, sh, F32, kind="ExternalInput").ap() for n, sh in PARAM_SHAPES}
    ropec = nc.dram_tensor("ropec", [S, 16], F32, kind="ExternalInput").ap()
    ropes = nc.dram_tensor("ropes", [S, 16], F32, kind="ExternalInput").ap()
    ropeT = nc.dram_tensor("ropeT", [2, 96, S], F32, kind="ExternalInput").ap()
    out = nc.dram_tensor("out", [S, D], F32, kind="ExternalOutput").ap()
    dk = "ExternalOutput" if debug else None

    def scr(name, shape, dt):
        if dk:
            return nc.dram_tensor(name, shape, dt, kind=dk).ap()
        return nc.dram_tensor(name, shape, dt).ap()
    tm_q = scr("tm_q", [S, 672], F32)
    tm_z = scr("tm_z", [S, 1024], F32)
    tm_dt = scr("tm_dt", [S, 32], F32)
    fm_xbc = scr("fm_xbc", [1536, S], BF16)
    fm_u = scr("fm_u", [512, S], BF16)
    fm_g = scr("fm_g", [3072, S], BF16)
    fm_xa = scr("fm_xa", [1536, S], BF16)
    fm_cv = scr("fm_cv", [512, S], BF16)
    fm_o = scr("fm_o", [512, S], BF16)
    fm_ys = scr("fm_ys", [1024, S], BF16)
    x1 = scr("x1", [S, D], F32)
    xmid = scr("xmid", [S, D], F32)
    Xg = scr("Xg", [NSLOT, D], BF16)
    Yg = scr("Yg", [NSLOT, D], F32)
    tm_slot = scr("tm_slot", [S, 4], I32)
    tm_gate = scr("tm_gate", [S, 4], F32)
    with contextlib.ExitStack() as st:
        P = Prog(nc, st)
        C = Ctx()
        setup_consts(nc, P, st, C)
        cur = x
        for l in range(depth):
            _LAYER[0] = l
            dst = out if l == depth - 1 else xmid
            phase_inproj(nc, P, C, S, cur, W["w_in"][l], W["b_in"][l], tm_q, tm_z, tm_dt, fm_xbc, fm_u, fm_g)
            if stop_after >= 2:
                phase_conv(nc, P, C, S, fm_xbc, fm_u, W["ssm_conv_w"][l], W["ssm_conv_b"][l], W["cnv_dw_w"][l],
                           W["cnv_dw_b"][l], W["cnv_ln_g"][l], W["cnv_ln_b"][l], fm_xa, fm_cv)
            if stop_after >= 3:
                phase_attn(nc, P, C, S, tm_q, W["mla_q_norm"][l], W["mla_kv_norm"][l], W["mla_w_uq"][l],
                           W["mla_w_ukv"][l], ropec, ropes, ropeT, fm_o, Xg=Xg)
            if stop_after >= 4 and 4 not in skip:
                phase_ssd(nc, P, C, S, fm_xa, tm_dt, tm_z, W["ssm_dt_bias"][l], W["ssm_a_log"][l], W["ssm_d"][l],
                          W["ssm_norm"][l], fm_ys)
            if stop_after >= 5 and 5 not in skip:
                phase_merge(nc, P, C, S, cur, fm_o, fm_ys, fm_cv, fm_g, W["w_br_attn"][l], W["w_br_ssm"][l],
                            W["w_br_conv"][l], W["b_br_conv"][l], W["w_out"][l], W["ln1_g"][l], W["ln1_b"][l],
                            W["router_w"][l], W["router_b"][l], x1, Xg, tm_slot, tm_gate)
            if stop_after >= 7 and 7 not in skip:
                phase_experts(nc, P, C, Xg, W["moe_w_gate_up"][l], W["moe_b_gate_up"][l], W["moe_w_down"][l],
                              W["moe_b_down"][l], Yg)
            if stop_after >= 8:
                phase_combine(nc, P, C, S, x1, Yg, tm_slot, tm_gate, W["ln2_g"][l], W["ln2_b"][l], dst)
            cur = dst
    return nc


def rope_tables(S):
    pos = np.arange(S, dtype=np.float32)
    inv = (np.float32(10000.0) ** (-np.arange(0, DR, 2, dtype=np.float32) / np.float32(DR))).astype(np.float32)
    ang = (pos[:, None] * inv[None, :]).astype(np.float32)
    c = np.cos(ang).astype(np.float32)
    s = np.sin(ang).astype(np.float32)
    rT = np.zeros((2, 96, S), np.float32)
    rT[0, 64:80] = c.T
    rT[0, 80:96] = c.T
    rT[1, 64:80] = s.T
    rT[1, 80:96] = s.T
    return c, s, rT


_NC_CACHE = {}


def kernel(**inputs):
    x = np.ascontiguousarray(inputs["x"], dtype=np.float32)
    B, S, _ = x.shape
    key = (S,)
    if key not in _NC_CACHE:
        _NC_CACHE[key] = build_program(S)
    nc = _NC_CACHE[key]
    c, s, rT = rope_tables(S)
    shared = {}
    for n, sh in PARAM_SHAPES:
        shared[n] = np.ascontiguousarray(inputs[n], dtype=np.float32).reshape(sh)
    shared.update(ropec=c, ropes=s, ropeT=rT)
    in_maps = [dict(shared, x=x[b]) for b in range(B)]
    res = run_bass_kernel_spmd(nc, in_maps, core_ids=list(range(B)))
    return np.stack([r["out"] for r in res.results], axis=0)
```

```python
import contextlib
import numpy as np
import concourse.bass as bass
import concourse.mybir as mybir
from concourse.bass_utils import run_bass_kernel_spmd

F32 = mybir.dt.float32
BF16 = mybir.dt.bfloat16
I32 = mybir.dt.int32
U32 = mybir.dt.uint32
AF = mybir.ActivationFunctionType
ALU = mybir.AluOpType
AX = mybir.AxisListType

D = 1024
DEPTH = 2
HEADS = 8
QL = 384
KVL = 256
DN = 64
DR = 32
DV = 64
SH = 16
SP = 64
DI = 1024
SG = 4
SN = 64
SCONV = 5
CDIM = 1536
CCH = 512
CW = 31
NE = 32
TOPK = 4
DFF = 1024
LIMIT = 7.0
SALPHA = 1.702
DN_ALPHA = (2 * DEPTH) ** 0.25
OFF_CQ, OFF_CKV, OFF_KR, OFF_Z, OFF_XBC, OFF_DT, OFF_CA, OFF_CG, OFF_GATE = (
    0, 384, 640, 672, 1696, 3232, 3264, 3776, 4288)
NIN = 7360


class _I:
    __slots__ = ("eng", "fn", "deps", "sig", "sigval", "dsem", "dval")

    def __init__(self, eng, fn, deps):
        self.eng = eng
        self.fn = fn
        self.deps = [d for d in deps if d is not None]
        self.sig = False
        self.sigval = 0
        self.dsem = None
        self.dval = 0


class DSem:
    def __init__(self, h):
        self.h = h
        self.count = 0


ENGS = ("pe", "act", "dve", "pool", "sp")


class Prog:
    def __init__(self, nc, stack):
        self.nc = nc
        self.esem = {e: stack.enter_context(nc.semaphore("es_" + e)) for e in ENGS}
        self.ecount = {e: 0 for e in ENGS}
        self.dsems = [DSem(stack.enter_context(nc.semaphore("ds%d" % i))) for i in range(40)]
        self.q = {e: [] for e in ENGS}
        self.used_dsems = set()
        self.regcache = {}

    def op(self, eng, fn, deps=()):
        i = _I(eng, fn, deps)
        self.q[eng].append(i)
        return i

    def dma(self, eng, out, in_, deps=(), sem=0, **kw):
        sem = sem + (20 if eng != "pool" else 0)
        ds = self.dsems[sem]
        i = _I(eng, lambda e: e.dma_start(out=out, in_=in_, **kw), deps)
        ds.count += 16
        i.dsem = ds
        i.dval = ds.count
        self.used_dsems.add(sem)
        self.q[eng].append(i)
        return i

    def idma(self, out, out_offset, in_, in_offset, deps=(), sem=0, **kw):
        ds = self.dsems[sem]
        bc = kw.pop("bounds_check", None)

        def fn(e):
            kw2 = dict(kw)
            if bc is not None:
                if bc not in self.regcache:
                    self.regcache[bc] = e.to_reg(bc)
                kw2["bounds_check"] = self.regcache[bc]
            return e.indirect_dma_start(out=out, out_offset=out_offset, in_=in_, in_offset=in_offset, **kw2)
        i = _I("pool", fn, deps)
        ds.count += 16
        i.dsem = ds
        i.dval = ds.count
        self.used_dsems.add(sem)
        self.q["pool"].append(i)
        return i

    def run(self):
        nc = self.nc
        for e in ENGS:
            for i in self.q[e]:
                for d in i.deps:
                    if d.dsem is None and (d.eng != i.eng or d.eng != "pe"):
                        d.sig = True
        for e in ENGS:
            c = self.ecount[e]
            for i in self.q[e]:
                if i.sig:
                    c += 1
                    i.sigval = c
            self.ecount[e] = c
        finals = [(ds.h, ds.count) for ds in (self.dsems[s] for s in sorted(self.used_dsems))]
        efinal = dict(self.ecount)

        def emit(eng_name, e):
            waited = {}
            for i in self.q[eng_name]:
                need = {}
                for d in i.deps:
                    if d.dsem is not None:
                        h, v = d.dsem.h, d.dval
                    else:
                        if not d.sig:
                            continue
                        h, v = self.esem[d.eng], d.sigval
                    key = id(h)
                    if key not in need or need[key][1] < v:
                        need[key] = (h, v)
                for key, (h, v) in need.items():
                    if waited.get(key, -1) >= v:
                        continue
                    waited[key] = v
                    e.wait_ge(h, v)
                ins = i.fn(e)
                if i.dsem is not None:
                    ins.then_inc(i.dsem.h, 16)
                elif i.sig:
                    ins.then_inc(self.esem[eng_name], 1)
            if eng_name == "sp":
                for h, v in finals:
                    e.wait_ge(h, v)
                for en in ("pe", "act", "dve", "pool"):
                    if efinal[en] > 0:
                        e.wait_ge(self.esem[en], efinal[en])

        with nc.allow_non_contiguous_dma(reason="small strided parameter loads"), nc.Block() as blk:
            @blk.tensor
            def _(e):
                emit("pe", e)

            @blk.scalar
            def _(e):
                emit("act", e)

            @blk.vector
            def _(e):
                emit("dve", e)

            @blk.gpsimd
            def _(e):
                emit("pool", e)

            @blk.sync
            def _(e):
                emit("sp", e)
        self.q = {e: [] for e in ENGS}
        self.used_dsems = set()
        self.regcache = {}

    def mm(self, out, lhsT, rhs, start=True, stop=True, deps=()):
        return self.op("pe", lambda e: e.matmul(out, lhsT=lhsT, rhs=rhs, start=start, stop=stop), deps)

    def tr(self, out, in_, ident, deps=()):
        return self.op("pe", lambda e: e.transpose(out, in_, ident), deps)

    def act(self, out, in_, func, bias=None, scale=1.0, deps=(), accum_out=None):
        kw = {}
        if bias is not None:
            kw["bias"] = bias
        if accum_out is not None:
            kw["accum_out"] = accum_out
        return self.op("act", lambda e: e.activation(out=out, in_=in_, func=func, scale=scale, **kw), deps)

    def tt(self, eng, out, in0, in1, op, deps=()):
        return self.op(eng, lambda e: e.tensor_tensor(out=out, in0=in0, in1=in1, op=op), deps)

    def ts(self, eng, out, in0, s1, s2=None, op0=ALU.mult, op1=None, deps=(), accum_out=None):
        kw = {}
        if op1 is not None:
            kw["op1"] = op1
        if accum_out is not None:
            kw["accum_out"] = accum_out
        return self.op(eng, lambda e: e.tensor_scalar(out=out, in0=in0, scalar1=s1, scalar2=s2, op0=op0, **kw), deps)

    def stt(self, out, in0, scalar, in1, op0, op1, deps=()):
        return self.op("dve", lambda e: e.scalar_tensor_tensor(out=out, in0=in0, scalar=scalar, in1=in1,
                                                               op0=op0, op1=op1), deps)

    def copy(self, eng, out, in_, deps=()):
        if eng == "act":
            return self.op("act", lambda e: e.copy(out=out, in_=in_), deps)
        return self.op(eng, lambda e: e.tensor_copy(out=out, in_=in_), deps)

    def memset(self, eng, ap, val, deps=()):
        return self.op(eng, lambda e: e.memset(ap, val), deps)


class Ring:
    def __init__(self, bufs, sem0=None):
        self.bufs = bufs
        self.n = len(bufs)
        self.k = 0
        self.readers = [[] for _ in bufs]
        self.sem0 = sem0

    def next(self):
        j = self.k % self.n
        self.k += 1
        r = self.readers[j]
        self.readers[j] = []
        return j, self.bufs[j], r

    def sem(self, j):
        return self.sem0 + j


def bcast_rows(ap1d, n, parts=128):
    return ap1d.partition_broadcast(parts)


_LAYER = [0]


class Ctx:
    pass


def setup_consts(nc, P, stack, C):
    C.ps = [stack.enter_context(nc.psum_tensor("psb%d" % i, [128, 512], F32)) for i in range(8)]
    C.ident_f = stack.enter_context(nc.sbuf_tensor("ident_f", [128, 128], F32))
    C.ident_b = stack.enter_context(nc.sbuf_tensor("ident_b", [128, 128], BF16))
    C.ones_b = stack.enter_context(nc.sbuf_tensor("ones_b", [128, 128], BF16))
    C.ones_f = stack.enter_context(nc.sbuf_tensor("ones_f", [128, 128], F32))
    C.m_le = stack.enter_context(nc.sbuf_tensor("m_le", [128, 128], F32))
    C.m_lt = stack.enter_context(nc.sbuf_tensor("m_lt", [128, 128], F32))
    C.m_ge = stack.enter_context(nc.sbuf_tensor("m_ge", [128, 128], F32))
    C.m_gt = stack.enter_context(nc.sbuf_tensor("m_gt", [128, 128], F32))
    C.m_gt_b = stack.enter_context(nc.sbuf_tensor("m_gt_b", [128, 128], BF16))
    C.m_lt_b = stack.enter_context(nc.sbuf_tensor("m_lt_b", [128, 128], BF16))
    a = P.memset("pool", C.ones_f[:], 1.0)
    b = P.memset("pool", C.ones_b[:], 1.0)
    P.op("pool", lambda e: e.affine_select(out=C.ident_f[:], in_=C.ones_f[:], pattern=[[-1, 128]],
                                           compare_op=ALU.is_equal, fill=0.0, base=0, channel_multiplier=1), [a])
    P.op("pool", lambda e: e.affine_select(out=C.ident_b[:], in_=C.ones_b[:], pattern=[[-1, 128]],
                                           compare_op=ALU.is_equal, fill=0.0, base=0, channel_multiplier=1), [b])
    P.op("pool", lambda e: e.affine_select(out=C.m_le[:], in_=C.ones_f[:], pattern=[[1, 128]],
                                           compare_op=ALU.is_ge, fill=0.0, base=0, channel_multiplier=-1), [a])
    P.op("pool", lambda e: e.affine_select(out=C.m_lt[:], in_=C.ones_f[:], pattern=[[1, 128]],
                                           compare_op=ALU.is_gt, fill=0.0, base=0, channel_multiplier=-1), [a])
    P.op("pool", lambda e: e.affine_select(out=C.m_ge[:], in_=C.ones_f[:], pattern=[[-1, 128]],
                                           compare_op=ALU.is_ge, fill=0.0, base=0, channel_multiplier=1), [a])
    P.op("pool", lambda e: e.affine_select(out=C.m_gt[:], in_=C.ones_f[:], pattern=[[-1, 128]],
                                           compare_op=ALU.is_gt, fill=0.0, base=0, channel_multiplier=1), [a])
    P.op("pool", lambda e: e.affine_select(out=C.m_gt_b[:], in_=C.ones_b[:], pattern=[[-1, 128]],
                                           compare_op=ALU.is_gt, fill=0.0, base=0, channel_multiplier=1), [b])
    P.op("pool", lambda e: e.affine_select(out=C.m_lt_b[:], in_=C.ones_b[:], pattern=[[1, 128]],
                                           compare_op=ALU.is_gt, fill=0.0, base=0, channel_multiplier=-1), [b])
    P.run()


def phase_inproj(nc, P, C, S, x, w_in, b_in, tm_q, tm_z, tm_dt, fm_xbc, fm_u, fm_g):
    with contextlib.ExitStack() as st:
        sb = lambda name, shape, dt: st.enter_context(nc.sbuf_tensor(name + "_L%d" % _LAYER[0], shape, dt))
        w = sb("p1_w", [128, 8, NIN], BF16)
        btm = sb("p1_btm", [128, 1728], F32)
        bfm = sb("p1_bfm", [128, 44], F32)
        xin_r = Ring([sb("p1_xin%d" % i, [128, D], F32) for i in range(2)], 2)
        xbf_r = Ring([sb("p1_xbf%d" % i, [128, D], BF16) for i in range(2)])
        xT_r = Ring([sb("p1_xT%d" % i, [128, 8, 512], BF16) for i in range(1)])
        otm_r = Ring([sb("p1_otm%d" % i, [128, 1728], F32) for i in range(1)], 4)
        ofm_r = Ring([sb("p1_ofm%d" % i, [128, 512], F32) for i in range(3)], 6)
        ogb_r = Ring([sb("p1_ogb%d" % i, [128, 512], BF16) for i in range(3)], 10)
        oa_r = Ring([sb("p1_oa%d" % i, [128, 512], F32) for i in range(2)])
        psr = Ring(C.ps[0:6])
        ptr = Ring(C.ps[6:8])

        wl = []
        for c in range(8):
            for j in range(4):
                wl.append(P.dma("pool", w[:, c, j * 1840:(j + 1) * 1840],
                                w_in[c * 128:(c + 1) * 128, j * 1840:(j + 1) * 1840], sem=0))
        bl = [P.dma("sp", btm[:, 0:1696], b_in[0:1696].partition_broadcast(128), sem=1),
              P.dma("sp", btm[:, 1696:1728], b_in[OFF_DT:OFF_DT + 32].partition_broadcast(128), sem=1)]
        fcols = [("x", i, OFF_XBC + 128 * i) for i in range(12)]
        for i in range(4):
            fcols += [("a", i, OFF_CA + 128 * i), ("g", i, OFF_CG + 128 * i)]
        fcols += [("s", i, OFF_GATE + 128 * i) for i in range(24)]
        for j, (_, _, co) in enumerate(fcols):
            bl.append(P.dma("sp", bfm[:, j:j + 1], b_in[co:co + 128].rearrange("(p o) -> p o", o=1), sem=1))

        nchunk = S // 512
        for ch in range(nchunk):
            _, xTt, xT_readers = xT_r.next()
            xT_w = []
            for tt in range(4):
                t0 = ch * 512 + tt * 128
                ji, xi, r1 = xin_r.next()
                ld = P.dma("sp", xi[:], x[t0:t0 + 128, :], deps=r1, sem=xin_r.sem(ji))
                jb, xb, r2 = xbf_r.next()
                cv = P.copy("act", xb[:], xi[:], deps=[ld] + r2)
                xin_r.readers[ji].append(cv)
                for kc in range(0, 8, 4):
                    jp, pt, r3 = ptr.next()
                    ptb = pt[:].bitcast(BF16)
                    trs = []
                    for q in range(4):
                        trs.append(P.tr(ptb[:, q * 128:(q + 1) * 128], xb[:, (kc + q) * 128:(kc + q + 1) * 128],
                                        C.ident_b[:], deps=[cv] + r3))
                    ev = P.copy("dve", xTt[:, kc:kc + 4, tt * 128:(tt + 1) * 128],
                                ptb[:, 0:512].rearrange("p (q t) -> p q t", q=4), deps=trs + xT_readers)
                    ptr.readers[jp].append(ev)
                    xbf_r.readers[jb].extend(trs)
                    xT_w.append(ev)
            rd = []
            for tt in range(4):
                t0 = ch * 512 + tt * 128
                jo, ot, r4 = otm_r.next()
                segs = [(0, 512, 0), (512, 160, 512), (OFF_Z, 512, 672), (OFF_Z + 512, 512, 1184), (OFF_DT, 32, 1696)]
                evs = []
                for (co, n, oo) in segs:
                    jp, pb, r5 = psr.next()
                    mms = []
                    for kc in range(8):
                        mms.append(P.mm(pb[:, 0:n], xTt[:, kc, tt * 128:(tt + 1) * 128], w[:, kc, co:co + n],
                                        start=(kc == 0), stop=(kc == 7), deps=xT_w + wl + r5))
                    rd.append(mms[-1])
                    ev = P.tt("dve", ot[:, oo:oo + n], pb[:, 0:n], btm[:, oo:oo + n], ALU.add,
                              deps=[mms[-1]] + bl + r4)
                    psr.readers[jp].append(ev)
                    evs.append(ev)
                sz = P.act(ot[:, 672:1696], ot[:, 672:1696], AF.Silu, deps=[evs[2], evs[3]])
                s1 = P.dma("pool", tm_q[t0:t0 + 128, :], ot[:, 0:672], deps=[evs[0], evs[1]], sem=otm_r.sem(jo))
                s2 = P.dma("pool", tm_z[t0:t0 + 128, :], ot[:, 672:1696], deps=[sz], sem=otm_r.sem(jo))
                s3 = P.dma("pool", tm_dt[t0:t0 + 128, :], ot[:, 1696:1728], deps=[evs[4]], sem=otm_r.sem(jo))
                otm_r.readers[jo].extend([s1, s2, s3])
            ts = slice(ch * 512, (ch + 1) * 512)
            a_cur = None
            for j, (kind, i, co) in enumerate(fcols):
                jp, pb, r5 = psr.next()
                mms = []
                for kc in range(8):
                    mms.append(P.mm(pb[:, :], w[:, kc, co:co + 128], xTt[:, kc, :],
                                    start=(kc == 0), stop=(kc == 7), deps=xT_w + wl + r5))
                rd.append(mms[-1])
                bcol = bfm[:, j:j + 1]
                if kind == "x":
                    jo, o, r6 = ogb_r.next()
                    ev = P.act(o[:], pb[:, :], AF.Identity, bias=bcol, deps=[mms[-1]] + bl + r6)
                    s = P.dma("pool", fm_xbc[i * 128:(i + 1) * 128, ts], o[:], deps=[ev], sem=ogb_r.sem(jo))
                    ogb_r.readers[jo].append(s)
                elif kind == "a":
                    ja, o, r6 = oa_r.next()
                    ev = P.act(o[:], pb[:, :], AF.Identity, bias=bcol, deps=[mms[-1]] + bl + r6)
                    a_cur = (o, ev, ja)
                elif kind == "g":
                    jo, o, r6 = ofm_r.next()
                    ev0 = P.act(o[:], pb[:, :], AF.Sigmoid, bias=bcol, deps=[mms[-1]] + bl + r6)
                    ao, aev, ja = a_cur
                    jo2, o2, r7 = ogb_r.next()
                    ev = P.tt("dve", o2[:], o[:], ao[:], ALU.mult, deps=[ev0, aev] + r7)
                    oa_r.readers[ja].append(ev)
                    ofm_r.readers[jo].append(ev)
                    s = P.dma("pool", fm_u[i * 128:(i + 1) * 128, ts], o2[:], deps=[ev], sem=ogb_r.sem(jo2))
                    ogb_r.readers[jo2].append(s)
                    ev = ev0
                else:
                    jo, o, r6 = ogb_r.next()
                    ev = P.act(o[:], pb[:, :], AF.Sigmoid, bias=bcol, deps=[mms[-1]] + bl + r6)
                    s = P.dma("pool", fm_g[i * 128:(i + 1) * 128, ts], o[:], deps=[ev], sem=ogb_r.sem(jo))
                    ogb_r.readers[jo].append(s)
                psr.readers[jp].append(ev)
            xT_r.readers[0].extend(rd)
        P.run()


def phase_conv(nc, P, C, S, fm_xbc, fm_u, sw, sbias, cw, cbias, lng, lnb, fm_xa, fm_cv):
    NB = S // 512
    with contextlib.ExitStack() as st:
        sb = lambda name, shape, dt: st.enter_context(nc.sbuf_tensor(name + "_L%d" % _LAYER[0], shape, dt))
        wts = sb("p2_w", [128, 12, SCONV], F32)
        bts = sb("p2_b", [128, 12], F32)
        wtc = sb("p2_wc", [128, 4, CW], F32)
        btc = sb("p2_bc", [128, 4], F32)
        gl = sb("p2_g", [128, 4], F32)
        bl_ = sb("p2_bl", [128, 4], F32)
        eps = sb("p2_eps", [128, 1], F32)
        dgs = sb("p2_dgs", [128, 12 * SCONV, 128], BF16)
        dgc = sb("p2_dgc", [128, 4 * CW, 128], BF16)
        xin_r = Ring([sb("p2_x%d" % i, [128, S + 32], BF16) for i in range(2)], 2)
        ob_r = Ring([sb("p2_o%d" % i, [128, S], BF16) for i in range(2)], 4)
        uc = sb("p2_uc", [128, 4, S], F32)
        sq_r = Ring([sb("p2_sq%d" % i, [128, 512], F32) for i in range(2)])
        mean = sb("p2_mean", [128, 512], F32)
        rstd = sb("p2_rstd", [128, 512], F32)
        tmp_r = Ring([sb("p2_t%d" % i, [128, 512], F32) for i in range(2)])
        ld = []
        for c in range(12):
            ld.append(P.dma("sp", wts[:, c, :], sw[:, c * 128:(c + 1) * 128].rearrange("k c -> c k"), sem=0))
            ld.append(P.dma("sp", bts[:, c:c + 1], sbias[c * 128:(c + 1) * 128].rearrange("(p o) -> p o", o=1), sem=0))
        for c in range(4):
            ld.append(P.dma("sp", wtc[:, c, :], cw[:, c * 128:(c + 1) * 128].rearrange("k c -> c k"), sem=0))
            for (t, src) in ((btc, cbias), (gl, lng), (bl_, lnb)):
                ld.append(P.dma("sp", t[:, c:c + 1], src[c * 128:(c + 1) * 128].rearrange("(p o) -> p o", o=1), sem=0))
        ld.append(P.memset("pool", eps[:], 1e-5))
        dgw = []
        for c in range(12):
            for k in range(SCONV):
                dgw.append(P.ts("dve", dgs[:, c * SCONV + k, :], C.ident_b[:], wts[:, c, k:k + 1], None, op0=ALU.mult,
                                deps=ld))
        for c in range(4):
            for k in range(CW):
                dgw.append(P.ts("dve", dgc[:, c * CW + k, :], C.ident_b[:], wtc[:, c, k:k + 1], None, op0=ALU.mult,
                                deps=ld))
        zs = []
        for b in xin_r.bufs:
            zs.append(P.memset("pool", b[:, 0:16], 0.0))
            zs.append(P.memset("pool", b[:, 16 + S:32 + S], 0.0))
        pcv = Ring(C.ps[4:8])
        for c in range(12):
            ji, xi, r1 = xin_r.next()
            l = P.dma("sp", xi[:, 16:16 + S], fm_xbc[c * 128:(c + 1) * 128, :], deps=r1 + zs, sem=xin_r.sem(ji))
            jo, ob, r3 = ob_r.next()
            evs = []
            for blk in range(NB):
                jp, pc, rp = pcv.next()
                mm = [P.mm(pc[:, :], dgs[:, c * SCONV + k, :], xi[:, 14 + k + blk * 512:14 + k + blk * 512 + 512],
                           start=(k == 0), stop=(k == SCONV - 1), deps=[l] + dgw + rp) for k in range(SCONV)]
                ev = P.act(ob[:, blk * 512:(blk + 1) * 512], pc[:, :], AF.Silu, bias=bts[:, c:c + 1],
                           deps=[mm[-1]] + r3 + ld)
                pcv.readers[jp].append(ev)
                xin_r.readers[ji].append(mm[-1])
                evs.append(ev)
            s = P.dma("pool", fm_xa[c * 128:(c + 1) * 128, :], ob[:], deps=evs, sem=ob_r.sem(jo))
            ob_r.readers[jo].append(s)
        xu = [sb("p2_xu%d" % i, [128, S + 32], BF16) for i in range(2)]
        xus = [xin_r.bufs[0], xin_r.bufs[1]] + xu
        lu = []
        for c in range(4):
            xi = xus[c]
            dz = []
            if c >= 2:
                dz = [P.memset("pool", xi[:, 0:16], 0.0), P.memset("pool", xi[:, 16 + S:32 + S], 0.0)]
                r1 = []
            else:
                _, _, r1 = xin_r.next()
            lu.append(P.dma("sp", xi[:, 16:16 + S], fm_u[c * 128:(c + 1) * 128, :], deps=r1 + zs + dz, sem=8 + c))
        psr = Ring(C.ps[0:4])
        last_norm = []
        for blk in range(NB):
            bs = slice(blk * 512, (blk + 1) * 512)
            ucw = []
            for c in range(4):
                xi = xus[c]
                jp, pc, rp = pcv.next()
                mm = [P.mm(pc[:, :], dgc[:, c * CW + k, :], xi[:, 1 + k + blk * 512:1 + k + blk * 512 + 512],
                           start=(k == 0), stop=(k == CW - 1), deps=[lu[c]] + dgw + rp) for k in range(CW)]
                ev = P.act(uc[:, c, bs], pc[:, :], AF.Identity, bias=btc[:, c:c + 1], deps=[mm[-1]] + ld)
                pcv.readers[jp].append(ev)
                ucw.append(ev)
            jp1, p_sum, r1 = psr.next()
            jp2, p_sq, r2 = psr.next()
            mm1 = []
            mm2 = []
            for c in range(4):
                mm1.append(P.mm(p_sum[:, :], C.ones_f[:], uc[:, c, bs], start=(c == 0), stop=(c == 3), deps=ucw + r1))
                js, sq, r3 = sq_r.next()
                sqi = P.act(sq[:], uc[:, c, bs], AF.Square, deps=ucw + r3)
                m = P.mm(p_sq[:, :], C.ones_f[:], sq[:], start=(c == 0), stop=(c == 3), deps=[sqi] + r2)
                sq_r.readers[js].append(m)
                mm2.append(m)
            e1 = P.ts("dve", mean[:], p_sum[:, :], 1.0 / CCH, None, op0=ALU.mult, deps=[mm1[-1]] + last_norm)
            psr.readers[jp1].append(e1)
            e2 = P.tt("dve", rstd[:], mean[:], mean[:], ALU.mult, deps=[e1] + last_norm)
            e3 = P.stt(rstd[:], p_sq[:, :], 1.0 / CCH, rstd[:], ALU.mult, ALU.subtract, deps=[e2, mm2[-1]])
            psr.readers[jp2].append(e3)
            e4 = P.act(rstd[:], rstd[:], AF.Sqrt, bias=eps[:, 0:1], deps=[e3])
            e5 = P.op("dve", lambda e, o=rstd: e.reciprocal(out=o[:], in_=o[:]), [e4])
            last_norm = []
            for c in range(4):
                jt, tmp, r4 = tmp_r.next()
                n1 = P.tt("dve", tmp[:], uc[:, c, bs], mean[:], ALU.subtract, deps=[e5] + r4)
                n2 = P.tt("dve", tmp[:], tmp[:], rstd[:], ALU.mult, deps=[n1])
                jo, ob, r5 = ob_r.next()
                n3 = P.act(ob[:, 0:512], tmp[:], AF.Silu, bias=bl_[:, c:c + 1], scale=gl[:, c:c + 1], deps=[n2] + r5)
                tmp_r.readers[jt].append(n3)
                s = P.dma("pool", fm_cv[c * 128:(c + 1) * 128, bs], ob[:, 0:512], deps=[n3], sem=ob_r.sem(jo))
                ob_r.readers[jo].append(s)
                last_norm.append(n2)
        P.run()


def rstd_from_ssq(P, ssq, n, eps_t, deps):
    a = P.act(ssq, ssq, AF.Sqrt, bias=eps_t, scale=1.0 / n, deps=deps)
    return P.op("dve", lambda e: e.reciprocal(out=ssq, in_=ssq), [a])


def phase_attn(nc, P, C, S, tm_q, qn_g, kvn_g, w_uq, w_ukv, ropec, ropes, ropeT, fm_o, Xg=None):
    NT = S // 128
    NQ = S // 512
    scale = float((DN + DR) ** -0.5)
    with contextlib.ExitStack() as st:
        sb = lambda name, shape, dt: st.enter_context(nc.sbuf_tensor(name + "_L%d" % _LAYER[0], shape, dt))
        wq = sb("p3_wq", [128, 3, 768], BF16)
        wqr = sb("p3_wqr", [128, 3, 768], BF16)
        wk = sb("p3_wk", [128, 2, 512], BF16)
        wv = sb("p3_wv", [128, 2, 512], BF16)
        gq = sb("p3_gq", [128, 384], F32)
        gkv = sb("p3_gkv", [128, 256], F32)
        eps = sb("p3_eps", [128, 1], F32)
        cqT = sb("p3_cqT", [128, 3, S], BF16)
        ckvT = sb("p3_ckvT", [128, 2, S], BF16)
        vaug = sb("p3_vaug", [128, NT, 8, 65], BF16)
        KT = [sb("p3_KT%d" % i, [96, S], BF16) for i in range(2)]
        QT = [sb("p3_QT%d" % i, [96, S], BF16) for i in range(2)]
        tin_r = Ring([sb("p3_tin%d" % i, [128, 672], F32) for i in range(2)], 2)
        cs_r = Ring([sb("p3_cs%d" % i, [128, 32], F32) for i in range(2)], 4)
        nb_r = Ring([sb("p3_nb%d" % i, [128, 768], BF16) for i in range(2)])
        junk = sb("p3_junk", [128, 384], F32)
        junkd = sb("p3_junkd", [128, 64], F32)
        ssq_r = Ring([sb("p3_ssq%d" % i, [128, 2], F32) for i in range(2)])
        rt_r = Ring([sb("p3_rt%d" % i, [96, 2, 512], F32) for i in range(2)], 6)
        rtmp = sb("p3_rtmp", [96, 2, 512], F32)
        pT_r = Ring([sb("p3_pT%d" % i, [128, 512], BF16) for i in range(4)])
        rs = sb("p3_rs", [65, 512], F32)
        bsb = sb("p3_bsb", [64, 512], F32)
        o_r = Ring([sb("p3_o%d" % i, [64, 512], BF16) for i in range(2)], 8)

        ld = []
        for kc in range(3):
            ld.append(P.dma("pool", wq[:, kc, :], w_uq[kc * 128:(kc + 1) * 128, :], sem=10))
        for kc in range(2):
            src = w_ukv[kc * 128:(kc + 1) * 128, :].rearrange("p (h t d) -> p h t d", t=2, d=64)
            ld.append(P.dma("pool", wk[:, kc, :].rearrange("p (h d) -> p h d", d=64), src[:, :, 0, :], sem=0))
            ld.append(P.dma("pool", wv[:, kc, :].rearrange("p (h d) -> p h d", d=64), src[:, :, 1, :], sem=0))
        ld.append(P.dma("sp", gq[:], qn_g.partition_broadcast(128), sem=1))
        ld.append(P.dma("sp", gkv[:], kvn_g.partition_broadcast(128), sem=1))
        ld.append(P.memset("pool", eps[:], 1e-6))
        z0 = P.memset("pool", wqr[:], 0.0)
        wq4 = wq[:].rearrange("p k (h d) -> p k h d", d=96)
        wqr4 = wqr[:].rearrange("p k (h d) -> p k h d", d=96)
        for kc in range(3):
            ld.append(P.ts("dve", wqr4[:, kc, :, 64:80], wq4[:, kc, :, 80:96], -1.0, None, op0=ALU.mult, deps=ld[0:3] + [z0]))
            ld.append(P.copy("dve", wqr4[:, kc, :, 80:96], wq4[:, kc, :, 64:80], deps=ld[0:3] + [z0]))
        ld.append(P.memset("pool", vaug[:, :, :, 64:65], 1.0))
        if Xg is not None:
            zt = sb("p3_zt", [128, 2, D], BF16)
            zm = P.memset("pool", zt[:], 0.0)
            xgv = Xg.rearrange("(g p) d -> p g d", p=128)
            for g0 in range(0, NSLOT // 128, 2):
                P.dma("pool", xgv[:, g0:g0 + 2, :], zt[:], deps=[zm], sem=18)

        psr = Ring(C.ps[0:4])
        ptr = Ring(C.ps[6:8])
        prep_w = []
        for t in range(NT):
            ts_ = slice(t * 128, (t + 1) * 128)
            ji, ti, r1 = tin_r.next()
            l1 = P.dma("sp", ti[:], tm_q[ts_, :], deps=r1, sem=tin_r.sem(ji))
            jc, cs, r2 = cs_r.next()
            l2 = P.dma("sp", cs[:, 0:16], ropec[ts_, :], deps=r2, sem=cs_r.sem(jc))
            l3 = P.dma("sp", cs[:, 16:32], ropes[ts_, :], deps=r2, sem=cs_r.sem(jc))
            js, ssq, r3 = ssq_r.next()
            a1 = P.act(junk[:, 0:384], ti[:, 0:384], AF.Square, accum_out=ssq[:, 0:1], deps=[l1] + r3 + ld)
            a2 = P.act(junk[:, 0:256], ti[:, 384:640], AF.Square, accum_out=ssq[:, 1:2], deps=[a1])
            b1 = rstd_from_ssq(P, ssq[:, 0:1], 384.0, eps[:, 0:1], [a1])
            b2 = rstd_from_ssq(P, ssq[:, 1:2], 256.0, eps[:, 0:1], [a2, b1])
            jn, nb, r4 = nb_r.next()
            n1 = P.stt(nb[:, 0:384], ti[:, 0:384], ssq[:, 0:1], gq[:], ALU.mult, ALU.mult, deps=[b1] + r4)
            n2 = P.stt(nb[:, 384:640], ti[:, 384:640], ssq[:, 1:2], gkv[:], ALU.mult, ALU.mult, deps=[b2] + r4)
            n3 = P.memset("pool", nb[:, 640:704], 0.0, deps=r4)
            k1, k2 = ti[:, 640:656], ti[:, 656:672]
            co, si = cs[:, 0:16], cs[:, 16:32]
            u1 = P.tt("dve", junkd[:, 0:16], k1, co, ALU.mult, deps=[l1, l2, l3, a2])
            u2 = P.tt("dve", junkd[:, 16:32], k2, si, ALU.mult, deps=[u1])
            u3 = P.tt("dve", nb[:, 704:720], junkd[:, 0:16], junkd[:, 16:32], ALU.subtract, deps=[u2] + r4)
            u4 = P.tt("dve", junkd[:, 32:48], k1, si, ALU.mult, deps=[u3])
            u5 = P.tt("dve", junkd[:, 48:64], k2, co, ALU.mult, deps=[u4])
            u6 = P.tt("dve", nb[:, 720:736], junkd[:, 32:48], junkd[:, 48:64], ALU.add, deps=[u5])
            tin_r.readers[ji].extend([n1, n2, u5])
            cs_r.readers[jc].append(u5)
            ssq_r.readers[js].extend([n1, n2])
            jp, pt, r5 = ptr.next()
            ptb = pt[:].bitcast(BF16)
            trs = []
            for q in range(3):
                trs.append(P.tr(ptb[:, q * 128:(q + 1) * 128], nb[:, q * 128:(q + 1) * 128], C.ident_b[:], deps=[n1] + r5))
            for q in range(2):
                trs.append(P.tr(ptb[:, (3 + q) * 128:(4 + q) * 128], nb[:, 384 + q * 128:384 + (q + 1) * 128],
                                C.ident_b[:], deps=[n2] + r5))
            trs.append(P.tr(ptb[0:96, 640:768], nb[:, 640:736], C.ident_b[:], deps=[u6, n3] + r5))
            e1 = P.copy("dve", cqT[:, :, ts_], ptb[:, 0:384].rearrange("p (q t) -> p q t", q=3), deps=trs)
            e2 = P.copy("dve", ckvT[:, :, ts_], ptb[:, 384:640].rearrange("p (q t) -> p q t", q=2), deps=trs)
            e3 = P.copy("dve", KT[0][64:96, ts_], ptb[64:96, 640:768], deps=trs)
            e4 = P.copy("dve", KT[1][64:96, ts_], ptb[64:96, 640:768], deps=trs)
            ptr.readers[jp].extend([e1, e2, e3, e4])
            nb_r.readers[jn].extend(trs)
            jv, pv, r6 = psr.next()
            mv = []
            for kc in range(2):
                mv.append(P.mm(pv[:, :], ckvT[:, kc, ts_], wv[:, kc, :], start=(kc == 0), stop=(kc == 1), deps=[e2] + ld + r6))
            e5 = P.copy("act", vaug[:, t, :, 0:64], pv[:, :].rearrange("p (h d) -> p h d", d=64), deps=[mv[-1]])
            psr.readers[jv].append(e5)
            prep_w.extend([e1, e2, e3, e4, e5])
        pso = Ring(C.ps[4:6])
        kq_readers = [[], []]
        deferred = []

        def emit_norm(h_, qs_, jo_, po_, last_):
            g1 = P.op("dve", lambda e, o=rs, i=po_: e.reciprocal(out=o[64:65, :], in_=i[64:65, :]), [last_])
            jb, pb, r4 = psr.next()
            g2 = P.mm(pb[0:64, :], C.ones_f[64:65, 0:64], rs[64:65, :], deps=[g1] + r4)
            g3 = P.copy("act", bsb[:], pb[0:64, :], deps=[g2])
            psr.readers[jb].append(g3)
            jo2, ob, r5 = o_r.next()
            g4 = P.tt("dve", ob[:], po_[0:64, :], bsb[:], ALU.mult, deps=[g3, last_] + r5)
            pso.readers[jo_].extend([g1, g4])
            s = P.dma("pool", fm_o[h_ * 64:(h_ + 1) * 64, qs_], ob[:], deps=[g4], sem=o_r.sem(jo2))
            o_r.readers[jo2].append(s)

        for h in range(HEADS):
            kt_, qt_ = KT[h % 2], QT[h % 2]
            hw = []
            for qc in range(NQ):
                qs = slice(qc * 512, (qc + 1) * 512)
                jk, pk, r1 = psr.next()
                mk = []
                for kc in range(2):
                    mk.append(P.mm(pk[0:64, :], wk[:, kc, h * 64:(h + 1) * 64], ckvT[:, kc, qs],
                                   start=(kc == 0), stop=(kc == 1), deps=prep_w + r1))
                ek = P.copy("dve", kt_[0:64, qs], pk[0:64, :], deps=[mk[-1]] + kq_readers[h % 2])
                psr.readers[jk].append(ek)
                jq, pq, r2 = psr.next()
                jr, pr, r3 = psr.next()
                mq = []
                mr = []
                for kc in range(3):
                    mq.append(P.mm(pq[0:96, :], wq[:, kc, h * 96:(h + 1) * 96], cqT[:, kc, qs],
                                   start=(kc == 0), stop=(kc == 2), deps=prep_w + r2))
                for kc in range(3):
                    mr.append(P.mm(pr[0:96, :], wqr[:, kc, h * 96:(h + 1) * 96], cqT[:, kc, qs],
                                   start=(kc == 0), stop=(kc == 2), deps=prep_w + r3))
                jt, rt, r4 = rt_r.next()
                lt = P.dma("sp", rt[64:96, :, :], ropeT[:, 64:96, qs].rearrange("t p s -> p t s"), deps=r4, sem=rt_r.sem(jt))
                eq = P.copy("act", qt_[0:64, qs], pq[0:64, :], deps=[mq[-1]] + kq_readers[h % 2])
                f1 = P.tt("dve", rtmp[64:96, 0, :], pq[64:96, :], rt[64:96, 0, :], ALU.mult, deps=[mq[-1], lt])
                f2 = P.tt("dve", rtmp[64:96, 1, :], pr[64:96, :], rt[64:96, 1, :], ALU.mult, deps=[mr[-1], lt, f1])
                f3 = P.tt("dve", qt_[64:96, qs], rtmp[64:96, 0, :], rtmp[64:96, 1, :], ALU.add,
                          deps=[f2] + kq_readers[h % 2])
                rt_r.readers[jt].extend([f1, f2])
                psr.readers[jq].extend([eq, f1])
                psr.readers[jr].append(f2)
                hw.extend([ek, eq, f3])
            kq_readers[h % 2] = []
            for qc in range(NQ):
                qs = slice(qc * 512, (qc + 1) * 512)
                jo, po, r1 = pso.next()
                last = None
                LA = 2
                pend = {}

                def issue_scores(kt):
                    js_, ps_s, r2 = psr.next()
                    m1 = P.mm(ps_s[:, :], kt_[0:96, kt * 128:(kt + 1) * 128], qt_[0:96, qs], deps=hw + prep_w + r2)
                    kq_readers[h % 2].append(m1)
                    pend[kt] = (js_, ps_s, m1)
                for kt in range(min(LA, NT)):
                    issue_scores(kt)
                for kt in range(NT):
                    js_, ps_s, m1 = pend.pop(kt)
                    jp_, pT, r3 = pT_r.next()
                    ex = P.act(pT[:], ps_s[:, :], AF.Exp, scale=scale, deps=[m1] + r3)
                    psr.readers[js_].append(ex)
                    if kt + LA < NT:
                        issue_scores(kt + LA)
                    m2 = P.mm(po[0:65, :], vaug[:, kt, h, :], pT[:], start=(kt == 0), stop=(kt == NT - 1),
                              deps=[ex] + r1)
                    pT_r.readers[jp_].append(m2)
                    last = m2
                    if kt == min(3, NT - 1) and deferred:
                        emit_norm(*deferred.pop(0))
                deferred.append((h, qs, jo, po, last))
        while deferred:
            emit_norm(*deferred.pop(0))
        P.run()


def phase_ssd(nc, P, C, S, fm_xa, tm_dt, tm_z, dt_bias, a_log, d_skip, norm_g, fm_ys):
    NT = S // 128
    with contextlib.ExitStack() as st:
        sb = lambda name, shape, dt: st.enter_context(nc.sbuf_tensor(name + "_L%d" % _LAYER[0], shape, dt))
        dtb = sb("p4_dtb", [128, 32], F32)
        At = sb("p4_A", [128, 32], F32)
        dsk16 = sb("p4_dsk16", [128, 16], F32)
        dsk = sb("p4_dsk", [128, 16, 64], F32)
        gt = sb("p4_gt", [128, 1024], F32)
        eps = sb("p4_eps", [128, 1], F32)
        Hin_b = sb("p4_Hinb", [128, NT, 512], BF16)
        Hst = [sb("p4_H%d" % d, [128, 2, 256], F32) for d in range(2)]
        Hbf = sb("p4_Hbf", [128, 2, 256], BF16)
        fmx_r = Ring([sb("p4_fmx%d" % i, [128, 12, 128], BF16) for i in range(2)], 2)
        dt_r = Ring([sb("p4_dt%d" % i, [128, 32], F32) for i in range(2)], 4)
        z_r = Ring([sb("p4_z%d" % i, [128, 1024], F32) for i in range(2)], 6)
        a_r = Ring([sb("p4_a%d" % i, [128, 32], F32) for i in range(2)])
        dtv_r = Ring([sb("p4_dtv%d" % i, [128, 32], F32) for i in range(2)])
        edec_r = Ring([sb("p4_edec%d" % i, [128, 96], F32) for i in range(2)])
        ws_r = Ring([sb("p4_ws%d" % i, [128, 32], F32) for i in range(2)])
        xs_r = Ring([sb("p4_xs%d" % i, [128, 1024], BF16) for i in range(2)])
        bt_r = Ring([sb("p4_bt%d" % i, [128, 256], BF16) for i in range(2)])
        xt_r = Ring([sb("p4_xt%d" % i, [128, 2, 1024], BF16) for i in range(2)])
        xd_r = Ring([sb("p4_xd%d" % i, [128, 2, 1024], BF16) for i in range(2)])
        gm_r = Ring([sb("p4_gm%d" % i, [128, 2, 4, 128], F32) for i in range(2)])
        rseg_r = Ring([sb("p4_rseg%d" % i, [128, 2, 4, 128], BF16) for i in range(3)])
        ahl_r = Ring([sb("p4_ahl%d" % i, [128, 2, 32], F32) for i in range(2)])
        ahb_r = Ring([sb("p4_ahb%d" % i, [128, 32], BF16) for i in range(2)])
        E_r = Ring([sb("p4_E%d" % i, [128, 4, 128], F32) for i in range(2)])
        MT_r = Ring([sb("p4_MT%d" % i, [128, 4, 128], BF16) for i in range(3)])
        E2_r = Ring([sb("p4_E2%d" % i, [128, 4, 128], F32) for i in range(2)])
        CTd_rp = [Ring([sb("p4_CTd%d_%d" % (par, i), [128, 4, 128], BF16) for i in range(2)]) for par in range(2)]
        CTz_r = Ring([sb("p4_CTz%d" % i, [128, 4, 128], BF16) for i in range(2)])
        y_r = Ring([sb("p4_y%d" % i, [128, 1024], F32) for i in range(2)])
        yb_r = Ring([sb("p4_yb%d" % i, [128, 1024], BF16) for i in range(2)])
        yT_r = Ring([sb("p4_yT%d" % i, [128, 8, 128], BF16) for i in range(2)], 8)
        ssq_r = Ring([sb("p4_ssq%d" % i, [128, 4], F32) for i in range(2)])
        junk = sb("p4_junk", [128, 256], F32)

        ld = [P.dma("sp", dtb[:], dt_bias.partition_broadcast(128), sem=0),
              P.dma("sp", At[:], a_log.partition_broadcast(128), sem=0),
              P.dma("sp", dsk16[:], d_skip.partition_broadcast(128), sem=0),
              P.dma("sp", gt[:], norm_g.partition_broadcast(128), sem=0)]
        i1 = P.act(At[:], At[:], AF.Exp, deps=ld)
        i2 = P.ts("dve", At[:], At[:], -1.0, None, op0=ALU.mult, deps=[i1])
        i3 = P.copy("dve", dsk[:], dsk16[:].unsqueeze(2).to_broadcast([128, 16, 64]), deps=ld)
        i4 = P.memset("pool", eps[:], 1e-6)
        i5 = P.memset("pool", Hst[0][:], 0.0)
        i6 = P.memset("pool", Hst[1][:], 0.0)
        init = ld + [i2, i3, i4, i5, i6]
        for par in range(2):
            for b in CTd_rp[par].bufs:
                init.append(P.memset("pool", b[:], 0.0))
        for b in CTz_r.bufs:
            init.append(P.memset("pool", b[:], 0.0))

        ps_small, ps_G, ps_tr, ps_st = C.ps[0], C.ps[1], C.ps[2], C.ps[3]
        ps_seg = Ring(C.ps[4:6])
        ps_y = [C.ps[6], C.ps[7]]
        rd = {"small": [], "G": [], "tr": [], "st": [], "y": []}
        fmxv = fm_xa.rearrange("(c p) s -> p c s", p=128)
        fysv = fm_ys.rearrange("(c p) s -> p c s", p=128)

        def prep(c, full):
            cs = slice(c * 128, (c + 1) * 128)
            o = {}
            jf, fmx, r = fmx_r.next()
            l1 = P.dma("sp", fmx[:], fmxv[:, :, cs], deps=r, sem=fmx_r.sem(jf))
            jd, dtt, r = dt_r.next()
            l2 = P.dma("sp", dtt[:], tm_dt[cs, :], deps=r, sem=dt_r.sem(jd))
            jv, dtv, r = dtv_r.next()
            q1 = P.tt("dve", dtv[:], dtt[:], dtb[:], ALU.add, deps=[l2] + init + r)
            dt_r.readers[jd].append(q1)
            q2 = P.act(dtv[:], dtv[:], AF.Exp, deps=[q1])
            q3 = P.act(dtv[:], dtv[:], AF.Ln, bias=1.0, deps=[q2])
            ja, a, r = a_r.next()
            q4 = P.tt("dve", a[:], dtv[:], At[:], ALU.mult, deps=[q3] + r)
            jhb, ahb, r = ahb_r.next()
            h1 = P.copy("dve", ahb[:], a[:], deps=[q4] + r)
            jhl, ahl, r = ahl_r.next()
            h2 = P.copy("dve", ahl[:, 0, :], ahb[:], deps=[h1] + r)
            h3 = P.tt("dve", ahl[:, 1, :], a[:], ahl[:, 0, :], ALU.subtract, deps=[h2])
            ahb_r.readers[jhb].append(h2)
            m = [P.mm(ps_small[:, 0:16], C.m_gt[:], a[:, 0:16], start=True, stop=False, deps=[q4] + rd["small"]),
                 P.mm(ps_small[:, 16:32], C.m_lt[:], a[:, 16:32], start=False, stop=False, deps=[q4]),
                 P.mm(ps_small[:, 32:48], C.m_le[:], a[:, 0:16], start=False, stop=False, deps=[q4]),
                 P.mm(ps_small[:, 48:64], C.m_ge[:], a[:, 16:32], start=False, stop=False, deps=[q4]),
                 P.mm(ps_small[:, 64:96], C.ones_f[:], a[:, 0:32], start=False, stop=True, deps=[q4])]
            je, edec, r = edec_r.next()
            q5 = P.act(edec[:], ps_small[:, 0:96], AF.Exp, deps=[m[-1]] + r)
            rd["small"] = [q5]
            jw, ws, r = ws_r.next()
            q6 = P.tt("dve", ws[:], dtv[:], edec[:, 0:32], ALU.mult, deps=[q5] + r)
            ptb = ps_tr[:].bitcast(BF16)
            jx, xs, r = xs_r.next()
            trs = [P.tr(ptb[:, q * 128:(q + 1) * 128], fmx[:, q, :], C.ident_b[:], deps=[l1] + rd["tr"]) for q in range(8)]
            q7 = P.copy("act", xs[:], ptb[:, :], deps=trs + r)
            jb, bt, r = bt_r.next()
            trs2 = [P.tr(ptb[:, q * 128:(q + 1) * 128], fmx[:, 8 + q, :], C.ident_b[:], deps=[l1, q7]) for q in range(2)]
            q8 = P.copy("act", bt[:], ptb[:, 0:256], deps=trs2 + r)
            rd["tr"] = [q8]
            xs3 = xs[:].rearrange("p (h d) -> p h d", d=64)
            jt, xt, r = xt_r.next()
            jq, xd, r2 = xd_r.next()
            xw = []
            for d in range(2):
                xw.append(P.tt("dve", xd[:, d, :].rearrange("p (h d) -> p h d", d=64), xs3,
                               ws[:, d * 16:(d + 1) * 16].unsqueeze(2).to_broadcast([128, 16, 64]), ALU.mult,
                               deps=[q6, q7] + r2))
                if full:
                    xw.append(P.tt("dve", xt[:, d, :].rearrange("p (h d) -> p h d", d=64), xs3,
                                   dtv[:, d * 16:(d + 1) * 16].unsqueeze(2).to_broadcast([128, 16, 64]), ALU.mult,
                                   deps=[q3, q7] + r))
            o.update(ahl=ahl, jhl=jhl, h3=h3)
            o.update(fmx=fmx, jf=jf, a=a, ja=ja, edec=edec, je=je, xs=xs, jx=jx, bt=bt, jb=jb, xt=xt, jt=jt,
                     xd=xd, jq=jq, dtv=dtv, jv=jv, ws=ws, jw=jw, ready=[l1, q4, q5, q6, q7, q8] + xw)
            return o

        def state_update(o, d, c_store=None):
            H = Hst[d]
            ups = []
            for j in range(2):
                m = P.mm(ps_st[:, :], o["bt"][:, j * 128:(j + 1) * 128], o["xd"][:, d, j * 512:(j + 1) * 512],
                         deps=o["ready"] + rd["st"])
                u = []
                for half in range(2):
                    g = 2 * j + half
                    hs = slice(half * 64, half * 64 + 64)
                    cd = o["edec"][hs, 64 + d * 16 + g * 4:64 + d * 16 + g * 4 + 4].unsqueeze(2).to_broadcast([64, 4, 64])
                    Hv = H[hs, j, :].rearrange("p (r d) -> p r d", d=64)
                    u1 = P.tt("dve", Hv, Hv, cd, ALU.mult, deps=o["ready"] + rd["H%d" % d])
                    u2 = P.tt("dve", H[hs, j, :], H[hs, j, :], ps_st[hs, half * 256:half * 256 + 256], ALU.add,
                              deps=[u1, m])
                    u.append(u2)
                rd["st"] = u
                ups.extend(u)
            return ups

        rd["H0"] = []
        rd["H1"] = []
        hb_w = [None] * NT
        prevu = [i6]
        for c in range(NT - 1, -1, -1):
            s0 = P.copy("act", Hin_b[:, c, :], Hst[1][:].rearrange("p j f -> p (j f)"), deps=prevu)
            hb_w[c] = s0
            rd["H1"] = [s0]
            if c > 0:
                o = prep(c, False)
                prevu = state_update(o, 1)
                for k in ("fmx", "a", "edec", "xs", "bt", "xd", "dtv", "ws"):
                    pass
                fmx_r.readers[o["jf"]].extend(o["ready"])
                a_r.readers[o["ja"]].extend(o["ready"])
                edec_r.readers[o["je"]].extend(prevu)
                xs_r.readers[o["jx"]].extend(o["ready"])
                bt_r.readers[o["jb"]].extend(prevu)
                xd_r.readers[o["jq"]].extend(prevu)
                dtv_r.readers[o["jv"]].extend(o["ready"])
                ws_r.readers[o["jw"]].extend(o["ready"])
        prevu = [i5]
        hbf_rd = []
        for c in range(NT):
            cs = slice(c * 128, (c + 1) * 128)
            o = prep(c, True)
            fmx, a, edec = o["fmx"], o["a"], o["edec"]
            rdy = o["ready"]
            hc = P.copy("act", Hbf[:].rearrange("p j f -> p (j f)"), Hst[0][:].rearrange("p j f -> p (j f)"),
                        deps=prevu + hbf_rd)
            rd["H0"] = [hc]
            hbf_rd = []
            gms = []
            jg, gm, r = gm_r.next()
            jz, CTz, rz_ = CTz_r.next()
            czw = []
            for g in range(4):
                hs = slice((g % 2) * 64, (g % 2) * 64 + 64)
                czw.append(P.copy("pool", CTz[hs, g, :], fmx[hs, 10 + g // 2, :], deps=rdy + init + rz_))
            for g in range(4):
                gms.append(P.mm(ps_G[:, g * 128:(g + 1) * 128], fmx[:, 8 + g // 2, :], CTz[:, g, :],
                                start=(g == 0), stop=(g == 3), deps=rdy + czw + rd["G"]))
            CTz_r.readers[jz].extend(gms)
            ge = []
            for g in range(4):
                ge.append(P.tt("dve", gm[:, 0, g, :], ps_G[:, g * 128:(g + 1) * 128], C.m_le[:], ALU.mult, deps=gms + r))
                ge.append(P.tt("dve", gm[:, 1, g, :], ps_G[:, g * 128:(g + 1) * 128], C.m_ge[:], ALU.mult, deps=gms + r))
            rd["G"] = ge
            first_y = [True, True]
            ymm = []

            def stage_a(d, g):
                lhs_seg = C.m_gt if d == 0 else C.m_lt
                mask2 = C.m_le if d == 0 else C.m_ge
                lhs_seg = C.m_gt_b if d == 0 else C.m_lt_b
                jr, rseg, r = rseg_r.next()
                rs_w = []
                for rr in range(4):
                    hcol = d * 16 + g * 4 + rr
                    for hl in range(2):
                        rs_w.append(P.ts("dve", rseg[:, hl, rr, :], mask2[:], o["ahl"][:, hl, hcol:hcol + 1], None,
                                         op0=ALU.mult, deps=rdy + [o["h3"]] + r))
                jp, pseg, r2 = ps_seg.next()
                P.mm(pseg[:, :], lhs_seg[:], rseg[:, 0].rearrange("p r l -> p (r l)"), start=True, stop=False,
                     deps=rs_w + r2)
                m1 = P.mm(pseg[:, :], lhs_seg[:], rseg[:, 1].rearrange("p r l -> p (r l)"), start=False, stop=True,
                          deps=rs_w)
                jE, E, r3 = E_r.next()
                x1 = P.act(E[:].rearrange("p r l -> p (r l)"), pseg[:, :], AF.Exp, deps=[m1] + r3)
                ps_seg.readers[jp].append(x1)
                jp2, pseg2, r5 = ps_seg.next()
                P.mm(pseg2[:, :], C.ones_b[:], rseg[:, 0].rearrange("p r l -> p (r l)"), start=True, stop=False,
                     deps=rs_w + r5)
                m2 = P.mm(pseg2[:, :], C.ones_b[:], rseg[:, 1].rearrange("p r l -> p (r l)"), start=False, stop=True,
                          deps=rs_w)
                rseg_r.readers[jr].extend([m1, m2])
                hs = slice((g % 2) * 64, (g % 2) * 64 + 64)
                jE2, E2, r6 = E2_r.next()
                x3 = P.act(E2[hs].rearrange("p r l -> p (r l)"), pseg2[hs, :], AF.Exp, deps=[m2] + r6)
                ps_seg.readers[jp2].append(x3)
                return dict(jE=jE, E=E, x1=x1, jE2=jE2, E2=E2, x3=x3)

            def stage_b(d, g, A):
                hs = slice((g % 2) * 64, (g % 2) * 64 + 64)
                jM, MT, r4 = MT_r.next()
                x2 = P.tt("dve", MT[:], A["E"][:], gm[:, d, g, :].unsqueeze(1).to_broadcast([128, 4, 128]), ALU.mult,
                          deps=[A["x1"]] + ge + r4)
                E_r.readers[A["jE"]].append(x2)
                CTd_r = CTd_rp[g % 2]
                jC, CTd, r7 = CTd_r.next()
                x4 = P.tt("dve", CTd[hs], A["E2"][hs], fmx[hs, 10 + g // 2, :].unsqueeze(1).to_broadcast([64, 4, 128]),
                          ALU.mult, deps=[A["x3"]] + r7 + init)
                E2_r.readers[A["jE2"]].append(x4)
                if d == 0:
                    hsrc = Hbf[:, g // 2, :]
                    hdeps = [hc]
                else:
                    hsrc = Hin_b[:, c, (g // 2) * 256:(g // 2) * 256 + 256]
                    hdeps = [hb_w[c]]
                for rr in range(4):
                    h = g * 4 + rr
                    bank = ps_y[h // 8]
                    col = (h % 8) * 64
                    ya = P.mm(bank[:, col:col + 64], MT[:, rr, :], o["xt"][:, d, h * 64:(h + 1) * 64],
                              start=first_y[h // 8], stop=False, deps=[x2] + rdy + rd["y"])
                    first_y[h // 8] = False
                    last = (d == 1 and g == 3 and rr == 3) or (d == 1 and g == 1 and rr == 3)
                    yo = P.mm(bank[:, col:col + 64], CTd[:, rr, :], hsrc[:, rr * 64:(rr + 1) * 64],
                              start=False, stop=last, deps=[x4] + hdeps)
                    ymm.extend([ya, yo])
                    MT_r.readers[jM].append(ya)
                    CTd_r.readers[jC].append(yo)
                    if d == 0:
                        hbf_rd.append(yo)

            its = [(d, g) for d in range(2) for g in range(4)]
            Acur = stage_a(*its[0])
            for ii, (d, g) in enumerate(its):
                Anext = stage_a(*its[ii + 1]) if ii + 1 < len(its) else None
                stage_b(d, g, Acur)
                Acur = Anext
            gm_r.readers[jg].extend(ymm)
            prevu = state_update(o, 0)
            jy, y, r = y_r.next()
            jz, z, rz = z_r.next()
            lz = P.dma("sp", z[:], tm_z[cs, :], deps=rz, sem=z_r.sem(jz))
            e1 = P.tt("dve", y[:].rearrange("p (h d) -> p h d", d=64), o["xs"][:].rearrange("p (h d) -> p h d", d=64),
                      dsk[:], ALU.mult, deps=rdy + init + r)
            e2 = P.tt("dve", y[:, 0:512], ps_y[0][:, :], y[:, 0:512], ALU.add, deps=[e1] + ymm)
            e3 = P.tt("dve", y[:, 512:1024], ps_y[1][:, :], y[:, 512:1024], ALU.add, deps=[e1] + ymm)
            rd["y"] = [e2, e3]
            e4 = P.tt("dve", y[:], y[:], z[:], ALU.mult, deps=[e2, e3, lz])
            z_r.readers[jz].append(e4)
            js, ssq, r = ssq_r.next()
            sq = []
            for g in range(4):
                sq.append(P.act(junk[:], y[:, g * 256:(g + 1) * 256], AF.Square, accum_out=ssq[:, g:g + 1],
                                deps=[e4] + r + sq[-1:]))
            f1 = P.act(ssq[:], ssq[:], AF.Sqrt, bias=eps[:, 0:1], scale=1.0 / 256.0, deps=sq)
            f2 = P.op("dve", lambda e, o_=ssq: e.reciprocal(out=o_[:], in_=o_[:]), [f1])
            jyb, yb, r = yb_r.next()
            f3 = []
            for g in range(4):
                f3.append(P.stt(yb[:, g * 256:(g + 1) * 256], y[:, g * 256:(g + 1) * 256], ssq[:, g:g + 1],
                                gt[:, g * 256:(g + 1) * 256], ALU.mult, ALU.mult, deps=[f2] + r))
            y_r.readers[jy].extend(f3)
            ssq_r.readers[js].extend(f3)
            ptb = ps_tr[:].bitcast(BF16)
            trs = [P.tr(ptb[:, q * 128:(q + 1) * 128], yb[:, q * 128:(q + 1) * 128], C.ident_b[:], deps=f3 + rd["tr"])
                   for q in range(8)]
            yb_r.readers[jyb].extend(trs)
            jT, yT, r = yT_r.next()
            f4 = P.copy("act", yT[:].rearrange("p q t -> p (q t)"), ptb[:, :], deps=trs + r)
            rd["tr"] = [f4]
            s = P.dma("pool", fysv[:, :, cs], yT[:], deps=[f4], sem=yT_r.sem(jT))
            yT_r.readers[jT].append(s)
            allr = ymm + prevu + [e1]
            fmx_r.readers[o["jf"]].extend(allr)
            a_r.readers[o["ja"]].extend(allr)
            edec_r.readers[o["je"]].extend(allr)
            xs_r.readers[o["jx"]].extend(allr)
            bt_r.readers[o["jb"]].extend(allr)
            xd_r.readers[o["jq"]].extend(allr)
            xt_r.readers[o["jt"]].extend(allr)
            dtv_r.readers[o["jv"]].extend(allr)
            ws_r.readers[o["jw"]].extend(allr)
            ahl_r.readers[o["jhl"]].extend(allr)
        P.run()


CAP = 640
NSLOT = NE * CAP


def layernorm_tile(P, v, stats, mv, g_t, b_t, eps_t, deps):
    s1 = P.op("dve", lambda e: e.bn_stats(out=stats[:, 0:6], in_=v[:, 0:512]), deps)
    s2 = P.op("dve", lambda e: e.bn_stats(out=stats[:, 6:12], in_=v[:, 512:1024]), deps)
    s3 = P.op("dve", lambda e: e.bn_aggr(out=mv[:, 0:2], in_=stats[:, 0:12]), [s1, s2])
    s4 = P.act(mv[:, 1:2], mv[:, 1:2], AF.Sqrt, bias=eps_t, deps=[s3])
    s5 = P.op("dve", lambda e: e.reciprocal(out=mv[:, 1:2], in_=mv[:, 1:2]), [s4])
    s6 = P.ts("dve", v, v, mv[:, 0:1], mv[:, 1:2], op0=ALU.subtract, op1=ALU.mult, deps=[s5])
    s7 = P.tt("dve", v, v, g_t, ALU.mult, deps=[s6])
    return P.tt("dve", v, v, b_t, ALU.add, deps=[s7])


def phase_merge(nc, P, C, S, x, fm_o, fm_ys, fm_cv, fm_g, w_ba, w_bs, w_bc, b_bc, w_out, ln_g, ln_b,
                router_w, router_b, x1, Xg, tm_slot, tm_gate):
    NQ = S // 512
    with contextlib.ExitStack() as st:
        sb = lambda name, shape, dt: st.enter_context(nc.sbuf_tensor(name + "_L%d" % _LAYER[0], shape, dt))
        wa = sb("p5_wa", [128, 4, D], BF16)
        ws_ = sb("p5_ws", [128, 8, D], BF16)
        wc = sb("p5_wc", [128, 4, D], BF16)
        wo = sb("p5_wo", [128, 8, D], BF16)
        wr = sb("p5_wr", [128, 8, NE], F32)
        bc = sb("p5_bc", [128, 8], F32)
        lg = sb("p5_lg", [128, D], F32)
        lb = sb("p5_lb", [128, D], F32)
        rb = sb("p5_rb", [128, NE], F32)
        eps = sb("p5_eps", [128, 1], F32)
        eiota = sb("p5_eiota", [128, NE], F32)
        cum = sb("p5_cum", [128, NE], F32)
        fo_r = Ring([sb("p5_fo%d" % i, [128, 4, 512], BF16) for i in range(1)], 2)
        fy_r = Ring([sb("p5_fy%d" % i, [128, 8, 512], BF16) for i in range(1)], 4)
        fc_r = Ring([sb("p5_fc%d" % i, [128, 4, 512], BF16) for i in range(1)], 6)
        fg_r = Ring([sb("p5_fg%d" % i, [128, 24, 512], BF16) for i in range(1)], 8)
        mix_r = Ring([sb("p5_mix%d" % i, [128, 8, 512], BF16) for i in range(2)])
        t_r = Ring([sb("p5_t%d" % i, [128, 512], F32) for i in range(3)])
        xr_r = Ring([sb("p5_xr%d" % i, [128, D], F32) for i in range(2)], 10)
        v_r = Ring([sb("p5_v%d" % i, [128, D], F32) for i in range(2)], 12)
        vb_r = Ring([sb("p5_vb%d" % i, [128, D], BF16) for i in range(2)], 14)
        xT_r = Ring([sb("p5_xT%d" % i, [128, 8, 128], F32) for i in range(2)])
        sm_r = Ring([sb("p5_sm%d" % i, [128, 480], F32) for i in range(2)], 16)
        sl_r = Ring([sb("p5_sl%d" % i, [128, 4], I32) for i in range(2)], 18)
        stats = sb("p5_stats", [128, 12], F32)
        mv = sb("p5_mv", [128, 2], F32)

        ld = []
        for kc in range(4):
            ld.append(P.dma("pool", wa[:, kc, :], w_ba[kc * 128:(kc + 1) * 128, :], sem=0))
            ld.append(P.dma("pool", wc[:, kc, :], w_bc[kc * 128:(kc + 1) * 128, :], sem=0))
        for kc in range(8):
            ld.append(P.dma("pool", ws_[:, kc, :], w_bs[kc * 128:(kc + 1) * 128, :], sem=0))
            ld.append(P.dma("pool", wo[:, kc, :], w_out[kc * 128:(kc + 1) * 128, :], sem=0))
            ld.append(P.dma("sp", wr[:, kc, :], router_w[kc * 128:(kc + 1) * 128, :], sem=1))
            ld.append(P.dma("sp", bc[:, kc:kc + 1], b_bc[kc * 128:(kc + 1) * 128].rearrange("(p o) -> p o", o=1), sem=1))
        ld.append(P.dma("sp", lg[:], ln_g.partition_broadcast(128), sem=1))
        ld.append(P.dma("sp", lb[:], ln_b.partition_broadcast(128), sem=1))
        ld.append(P.dma("sp", rb[:], router_b.partition_broadcast(128), sem=1))
        ld.append(P.memset("pool", eps[:], 1e-5))
        ld.append(P.memset("pool", cum[:], 0.0))
        ld.append(P.op("pool", lambda e: e.iota(eiota[:], pattern=[[CAP, NE]], base=0, channel_multiplier=0,
                                                allow_small_or_imprecise_dtypes=True)))
        zf = []
        fov = fm_o.rearrange("(c p) s -> p c s", p=128)
        fyv = fm_ys.rearrange("(c p) s -> p c s", p=128)
        fcv = fm_cv.rearrange("(c p) s -> p c s", p=128)
        fgv = fm_g.rearrange("(c p) s -> p c s", p=128)
        psr = Ring(C.ps[0:4])
        pso = Ring(C.ps[4:6])
        ps_t = C.ps[6]
        ps_r = C.ps[7]
        rd_t = []
        rd_r = []
        cum_w = []
        for qc in range(NQ):
            qs = slice(qc * 512, (qc + 1) * 512)
            jo, fo, r = fo_r.next()
            l1 = P.dma("sp", fo[:], fov[:, :, qs], deps=r, sem=fo_r.sem(jo))
            jy, fy, r = fy_r.next()
            l2 = P.dma("sp", fy[:], fyv[:, :, qs], deps=r, sem=fy_r.sem(jy))
            jc, fc, r = fc_r.next()
            l3 = P.dma("sp", fc[:], fcv[:, :, qs], deps=r, sem=fc_r.sem(jc))
            jg, fg, r = fg_r.next()
            l4 = P.dma("sp", fg[:], fgv[:, :, qs], deps=r, sem=fg_r.sem(jg))
            jm, mix, rmix = mix_r.next()
            lds = [l1, l2, l3, l4] + ld
            mixw = []
            allmm = []
            for f in range(8):
                fs = slice(f * 128, (f + 1) * 128)
                ja, pa, r = psr.next()
                ma = [P.mm(pa[:, :], wa[:, kc, fs], fo[:, kc, :], start=(kc == 0), stop=(kc == 3), deps=lds + r)
                      for kc in range(4)]
                jt1, t1, r = t_r.next()
                e1 = P.tt("dve", t1[:], pa[:, :], fg[:, f, :], ALU.mult, deps=[ma[-1]] + r)
                psr.readers[ja].append(e1)
                js, pss, r = psr.next()
                ms = [P.mm(pss[:, :], ws_[:, kc, fs], fy[:, kc, :], start=(kc == 0), stop=(kc == 7), deps=lds + r)
                      for kc in range(8)]
                jt2, t2, r = t_r.next()
                e2 = P.tt("dve", t2[:], pss[:, :], fg[:, 8 + f, :], ALU.mult, deps=[ms[-1]] + r)
                psr.readers[js].append(e2)
                e3 = P.tt("dve", t1[:], t1[:], t2[:], ALU.add, deps=[e1, e2])
                t_r.readers[jt2].append(e3)
                jc2, pc, r = psr.next()
                mc = [P.mm(pc[:, :], wc[:, kc, fs], fc[:, kc, :], start=(kc == 0), stop=(kc == 3), deps=lds + r)
                      for kc in range(4)]
                jt3, t3, r = t_r.next()
                e4 = P.stt(t3[:], pc[:, :], bc[:, f:f + 1], fg[:, 16 + f, :], ALU.add, ALU.mult, deps=[mc[-1]] + r)
                psr.readers[jc2].append(e4)
                e5 = P.tt("dve", mix[:, f, :], t1[:], t3[:], ALU.add, deps=[e3, e4] + rmix)
                t_r.readers[jt1].append(e5)
                t_r.readers[jt3].append(e5)
                mixw.append(e5)
                allmm.extend([ma[-1], ms[-1], mc[-1]])
            fo_r.readers[jo].extend(allmm)
            fy_r.readers[jy].extend(allmm)
            fc_r.readers[jc].extend(allmm)
            fg_r.readers[jg].extend(mixw)
            for tt in range(4):
                t0 = qc * 512 + tt * 128
                tsl = slice(t0, t0 + 128)
                jx, xr, r = xr_r.next()
                lx = P.dma("sp", xr[:], x[tsl, :], deps=r, sem=xr_r.sem(jx))
                jv, v, rv = v_r.next()
                ev = []
                for half in range(2):
                    jp, po, r = pso.next()
                    mo = [P.mm(po[:, :], mix[:, f, tt * 128:(tt + 1) * 128], wo[:, f, half * 512:(half + 1) * 512],
                               start=(f == 0), stop=(f == 7), deps=mixw + ld + r) for f in range(8)]
                    e = P.stt(v[:, half * 512:(half + 1) * 512], xr[:, half * 512:(half + 1) * 512], float(DN_ALPHA),
                              po[:, :], ALU.mult, ALU.add, deps=[mo[-1], lx] + rv)
                    pso.readers[jp].append(e)
                    mix_r.readers[jm].append(mo[-1])
                    ev.append(e)
                xr_r.readers[jx].extend(ev)
                lnl = layernorm_tile(P, v[:], stats, mv, lg[:], lb[:], eps[:, 0:1], ev + ld)
                s1 = P.dma("pool", x1[tsl, :], v[:], deps=[lnl], sem=v_r.sem(jv))
                jb, vb, r = vb_r.next()
                cvb = P.copy("act", vb[:], v[:], deps=[lnl] + r)
                jT, xT, rT = xT_r.next()
                tw = []
                for rnd in range(2):
                    trs = [P.tr(ps_t[:, q * 128:(q + 1) * 128], v[:, (rnd * 4 + q) * 128:(rnd * 4 + q + 1) * 128],
                                C.ident_f[:], deps=[lnl] + rd_t) for q in range(4)]
                    ec = P.copy("act", xT[:, rnd * 4:rnd * 4 + 4, :].rearrange("p q t -> p (q t)"), ps_t[:, :],
                                deps=trs + rT)
                    rd_t = [ec]
                    tw.append(ec)
                v_r.readers[jv].extend([s1, cvb] + tw)
                ml = [P.mm(ps_r[:, 0:NE], xT[:, kc, :], wr[:, kc, :], start=(kc == 0), stop=(kc == 7), deps=tw + ld + rd_r)
                      for kc in range(8)]
                xT_r.readers[jT].append(ml[-1])
                jsm, sm, r = sm_r.next()
                LG, MK, EX, PO, T1, T2 = (sm[:, 0:32], sm[:, 32:64], sm[:, 64:96], sm[:, 96:128], sm[:, 128:160],
                                          sm[:, 160:192])
                V8, NV0, ZS, SLF, GK = sm[:, 192:200], sm[:, 200:201], sm[:, 201:202], sm[:, 204:208], sm[:, 208:212]
                a1 = P.tt("dve", LG, ps_r[:, 0:NE], rb[:], ALU.add, deps=[ml[-1]] + r)
                a2 = P.op("dve", lambda e, o=V8, i=LG: e.max(out=o, in_=i), [a1])
                a3 = P.ts("dve", MK, LG, V8[:, 3:4], None, op0=ALU.is_ge, deps=[a2])
                a4 = P.ts("dve", NV0, V8[:, 0:1], -1.0, None, op0=ALU.mult, deps=[a2])
                a5 = P.act(EX, LG, AF.Exp, bias=NV0, deps=[a4])
                a6 = P.tt("dve", EX, EX, MK, ALU.mult, deps=[a5, a3])
                a7 = P.op("dve", lambda e, o=ZS, i=EX: e.reduce_sum(out=o, in_=i, axis=AX.X), [a6])
                a8 = P.op("dve", lambda e, o=ZS: e.reciprocal(out=o, in_=o), [a7])
                a9 = P.ts("dve", EX, EX, ZS, None, op0=ALU.mult, deps=[a8])
                mp1 = P.mm(ps_r[:, 64:64 + NE], C.m_lt[:], MK, start=True, stop=False, deps=[a3, a1])
                mp2 = P.mm(ps_r[:, 64:64 + NE], C.ident_f[:], cum[:], start=False, stop=False, deps=cum_w + ld)
                mp3 = P.mm(ps_r[:, 128:128 + NE], C.ones_f[:], MK, start=False, stop=True, deps=[a3])
                b1 = P.ts("dve", T1, ps_r[:, 64:64 + NE], float(CAP), None, op0=ALU.is_ge, deps=[mp2, mp3])
                b2 = P.tt("dve", PO, ps_r[:, 64:64 + NE], eiota[:], ALU.add, deps=[mp2, mp3])
                b3 = P.stt(PO, T1, 1.0e6, PO, ALU.mult, ALU.add, deps=[b1, b2])
                b4 = P.tt("dve", cum[:], cum[:], ps_r[:, 128:128 + NE], ALU.add, deps=[mp3, mp2])
                cum_w = [b4]
                b5 = P.ts("dve", T1, T1, -1.0, 1.0, op0=ALU.mult, op1=ALU.add, deps=[b3])
                b6 = P.tt("dve", EX, EX, T1, ALU.mult, deps=[b5, a9])
                rd_r = [a1, b1, b2, b4]
                T4 = sm[:, 212:212 + 128].rearrange("p (k e) -> p k e", k=4)
                c1 = P.tt("dve", T4, LG.unsqueeze(1).to_broadcast([128, 4, NE]),
                          V8[:, 0:4].unsqueeze(2).to_broadcast([128, 4, NE]), ALU.is_equal, deps=[b6])
                T5 = sm[:, 340:340 + 128].rearrange("p (k e) -> p k e", k=4)
                c2 = P.tt("dve", T5, T4, PO.unsqueeze(1).to_broadcast([128, 4, NE]), ALU.mult, deps=[c1])
                c3 = P.op("dve", lambda e, o=SLF, i=T5: e.reduce_sum(out=o, in_=i, axis=AX.X), [c2])
                c4 = P.tt("dve", T5, T4, EX.unsqueeze(1).to_broadcast([128, 4, NE]), ALU.mult, deps=[c3])
                lastk = P.op("dve", lambda e, o=GK, i=T5: e.reduce_sum(out=o, in_=i, axis=AX.X), [c4])
                jsl, sl, r = sl_r.next()
                d1 = P.copy("dve", sl[:], SLF, deps=[lastk] + r)
                sc = []
                for k in range(4):
                    sc.append(P.idma(Xg[:, :], bass.IndirectOffsetOnAxis(ap=sl[:, k:k + 1], axis=0), vb[:], None,
                                     deps=[d1, cvb] + zf, sem=vb_r.sem(jb), bounds_check=NSLOT - 1, oob_is_err=False))
                vb_r.readers[jb].extend(sc)
                s2 = P.dma("pool", tm_slot[tsl, :], sl[:], deps=[d1], sem=sl_r.sem(jsl))
                s3 = P.dma("pool", tm_gate[tsl, :], GK, deps=[lastk], sem=sm_r.sem(jsm))
                sl_r.readers[jsl].extend(sc + [s2])
                sm_r.readers[jsm].extend([s3, d1])
        P.run()


def phase_experts(nc, P, C, Xg, w_gu, b_gu, w_dn, b_dn, Yg):
    NJ = CAP // 128
    with contextlib.ExitStack() as st:
        sb = lambda name, shape, dt: st.enter_context(nc.sbuf_tensor(name + "_L%d" % _LAYER[0], shape, dt))
        wg_r = Ring([sb("p7_wg%d" % i, [128, 8, 2 * DFF], BF16) for i in range(2)], 0)
        wd_r = Ring([sb("p7_wd%d" % i, [128, 8, D], BF16) for i in range(2)], 2)
        bg_r = Ring([sb("p7_bg%d" % i, [128, 16], F32) for i in range(2)], 4)
        bd_r = Ring([sb("p7_bd%d" % i, [128, D], F32) for i in range(2)], 6)
        xs_r = Ring([sb("p7_xs%d" % i, [128, D], BF16) for i in range(3)], 8)
        xT_r = Ring([sb("p7_xT%d" % i, [128, 8, CAP], BF16) for i in range(2)])
        aT_r = Ring([sb("p7_aT%d" % i, [128, 8, CAP], BF16) for i in range(1)])
        g_r = Ring([sb("p7_g%d" % i, [128, 512], F32) for i in range(2)])
        s_r = Ring([sb("p7_s%d" % i, [128, 512], F32) for i in range(2)])
        u_r = Ring([sb("p7_u%d" % i, [128, 512], F32) for i in range(2)])
        y_r = Ring([sb("p7_y%d" % i, [128, D], F32) for i in range(2)], 11)
        psr = Ring(C.ps[0:6])
        ptr = Ring(C.ps[6:8])
        colgroups = []
        c0 = 0
        while c0 < CAP:
            n = min(512, CAP - c0)
            colgroups.append((c0, n))
            c0 += n
        def load_expert(e):
            jw, wg, r = wg_r.next()
            lw = []
            for kc in range(8):
                lw.append(P.dma("pool", wg[:, kc, :], w_gu[e, kc * 128:(kc + 1) * 128, :], deps=r, sem=wg_r.sem(jw)))
            jd, wd, r = wd_r.next()
            for kc in range(8):
                lw.append(P.dma("pool", wd[:, kc, :], w_dn[e, kc * 128:(kc + 1) * 128, :], deps=r, sem=wd_r.sem(jd)))
            jb, bg, r = bg_r.next()
            lw.append(P.dma("sp", bg[:], b_gu[e, :].rearrange("(c p) -> p c", p=128), deps=r, sem=bg_r.sem(jb)))
            jbd, bd, r = bd_r.next()
            lw.append(P.dma("sp", bd[:], b_dn[e, :].partition_broadcast(128), deps=r, sem=bd_r.sem(jbd)))
            return (jw, wg, jd, wd, jb, bg, jbd, bd, lw)

        nxt = load_expert(0)
        for e in range(NE):
            jw, wg, jd, wd, jb, bg, jbd, bd, lw = nxt
            jT, xT, rT = xT_r.next()
            xw = []
            for j in range(NJ):
                r0 = e * CAP + j * 128
                jx, xs, r = xs_r.next()
                lx = P.dma("sp", xs[:], Xg[r0:r0 + 128, :], deps=r, sem=xs_r.sem(jx))
                for kc in range(0, 8, 4):
                    jp, pt, r3 = ptr.next()
                    ptb = pt[:].bitcast(BF16)
                    trs = [P.tr(ptb[:, q * 128:(q + 1) * 128], xs[:, (kc + q) * 128:(kc + q + 1) * 128], C.ident_b[:],
                                deps=[lx] + r3) for q in range(4)]
                    ev = P.copy("dve", xT[:, kc:kc + 4, j * 128:(j + 1) * 128],
                                ptb[:, 0:512].rearrange("p (q t) -> p q t", q=4), deps=trs + rT)
                    ptr.readers[jp].append(ev)
                    xs_r.readers[jx].extend(trs)
                    xw.append(ev)
            if e + 1 < NE:
                nxt = load_expert(e + 1)
            ja, aT, ra = aT_r.next()
            aw = []
            hmm = []
            for f in range(8):
                for (c0, n) in colgroups:
                    jg, pg, r1 = psr.next()
                    mg = [P.mm(pg[:, 0:n], wg[:, kc, f * 128:(f + 1) * 128], xT[:, kc, c0:c0 + n],
                               start=(kc == 0), stop=(kc == 7), deps=xw + lw + r1) for kc in range(8)]
                    ju, pu, r2 = psr.next()
                    mu = [P.mm(pu[:, 0:n], wg[:, kc, DFF + f * 128:DFF + (f + 1) * 128], xT[:, kc, c0:c0 + n],
                               start=(kc == 0), stop=(kc == 7), deps=xw + lw + r2) for kc in range(8)]
                    hmm.extend([mg[-1], mu[-1]])
                    jgg, gt, r3 = g_r.next()
                    h1 = P.ts("dve", gt[:, 0:n], pg[:, 0:n], bg[:, f:f + 1], LIMIT, op0=ALU.add, op1=ALU.min,
                              deps=[mg[-1]] + r3)
                    psr.readers[jg].append(h1)
                    jss, sg, r4 = s_r.next()
                    h2 = P.act(sg[:, 0:n], gt[:, 0:n], AF.Sigmoid, scale=SALPHA, deps=[h1] + r4)
                    h3 = P.tt("dve", gt[:, 0:n], gt[:, 0:n], sg[:, 0:n], ALU.mult, deps=[h2])
                    s_r.readers[jss].append(h3)
                    juu, ut, r5 = u_r.next()
                    h4 = P.ts("dve", ut[:, 0:n], pu[:, 0:n], bg[:, 8 + f:9 + f], LIMIT, op0=ALU.add, op1=ALU.min,
                              deps=[mu[-1]] + r5)
                    psr.readers[ju].append(h4)
                    h5 = P.ts("dve", ut[:, 0:n], ut[:, 0:n], -LIMIT, 1.0, op0=ALU.max, op1=ALU.add, deps=[h4])
                    h6 = P.tt("dve", aT[:, f, c0:c0 + n], ut[:, 0:n], gt[:, 0:n], ALU.mult, deps=[h5, h3] + ra)
                    g_r.readers[jgg].append(h6)
                    u_r.readers[juu].append(h6)
                    aw.append(h6)
            xT_r.readers[jT].extend(hmm)
            wg_r.readers[jw].extend(hmm)
            bg_r.readers[jb].extend(aw)
            dmm = []
            yw = []
            for j in range(NJ):
                r0 = e * CAP + j * 128
                jy, y, ry = y_r.next()
                evs = []
                for half in range(2):
                    jp, pd, r1 = psr.next()
                    md = [P.mm(pd[:, :], aT[:, f, j * 128:(j + 1) * 128], wd[:, f, half * 512:(half + 1) * 512],
                               start=(f == 0), stop=(f == 7), deps=aw + lw + r1) for f in range(8)]
                    dmm.append(md[-1])
                    ev = P.tt("dve", y[:, half * 512:(half + 1) * 512], pd[:, :], bd[:, half * 512:(half + 1) * 512],
                              ALU.add, deps=[md[-1]] + ry)
                    psr.readers[jp].append(ev)
                    evs.append(ev)
                s = P.dma("pool", Yg[r0:r0 + 128, :], y[:], deps=evs, sem=y_r.sem(jy))
                y_r.readers[jy].append(s)
                yw.extend(evs)
            aT_r.readers[ja].extend(dmm)
            wd_r.readers[jd].extend(dmm)
            bd_r.readers[jbd].extend(yw)
        P.run()


def phase_combine(nc, P, C, S, x1, Yg, tm_slot, tm_gate, ln_g, ln_b, xout):
    NT = S // 128
    with contextlib.ExitStack() as st:
        sb = lambda name, shape, dt: st.enter_context(nc.sbuf_tensor(name + "_L%d" % _LAYER[0], shape, dt))
        lg = sb("p8_lg", [128, D], F32)
        lb = sb("p8_lb", [128, D], F32)
        eps = sb("p8_eps", [128, 1], F32)
        stats = sb("p8_stats", [128, 12], F32)
        mv = sb("p8_mv", [128, 2], F32)
        x_r = Ring([sb("p8_x%d" % i, [128, D], F32) for i in range(3)], 2)
        sl_r = Ring([sb("p8_sl%d" % i, [128, 4], I32) for i in range(3)], 5)
        gk_r = Ring([sb("p8_gk%d" % i, [128, 4], F32) for i in range(3)], 8)
        yk_r = Ring([sb("p8_yk%d" % i, [128, D], F32) for i in range(12)], 8)
        ld = [P.dma("sp", lg[:], ln_g.partition_broadcast(128), sem=0),
              P.dma("sp", lb[:], ln_b.partition_broadcast(128), sem=0),
              P.memset("pool", eps[:], 1e-5)]
        for b in yk_r.bufs:
            ld.append(P.memset("pool", b[:], 0.0))
        for t in range(NT):
            tsl = slice(t * 128, (t + 1) * 128)
            jx, xt, r = x_r.next()
            l1 = P.dma("sp", xt[:], x1[tsl, :], deps=r, sem=x_r.sem(jx))
            js, sl, r = sl_r.next()
            l2 = P.dma("sp", sl[:], tm_slot[tsl, :], deps=r, sem=sl_r.sem(js))
            jg, gk, r = gk_r.next()
            l3 = P.dma("sp", gk[:], tm_gate[tsl, :], deps=r, sem=gk_r.sem(jg))
            prev = P.ts("dve", xt[:], xt[:], float(DN_ALPHA), None, op0=ALU.mult, deps=[l1])
            gs = []
            for k in range(4):
                jy, yk, r = yk_r.next()
                g = P.idma(yk[:], None, Yg[:, :], bass.IndirectOffsetOnAxis(ap=sl[:, k:k + 1], axis=0),
                           deps=[l2] + r + ld, sem=yk_r.sem(jy), bounds_check=NSLOT - 1, oob_is_err=False)
                prev = P.stt(xt[:], yk[:], gk[:, k:k + 1], xt[:], ALU.mult, ALU.add, deps=[g, l3, prev])
                yk_r.readers[jy].append(prev)
                gs.append(g)
            sl_r.readers[js].extend(gs)
            gk_r.readers[jg].append(prev)
            lnl = layernorm_tile(P, xt[:], stats, mv, lg[:], lb[:], eps[:, 0:1], [prev] + ld)
            s = P.dma("act", xout[tsl, :], xt[:], deps=[lnl], sem=11 + jx)
            x_r.readers[jx].append(s)
        P.run()


PARAM_SHAPES = [
    ("w_in", [DEPTH, D, NIN]), ("b_in", [DEPTH, NIN]), ("mla_q_norm", [DEPTH, QL]), ("mla_kv_norm", [DEPTH, KVL]),
    ("mla_w_uq", [DEPTH, QL, HEADS * (DN + DR)]), ("mla_w_ukv", [DEPTH, KVL, HEADS * (DN + DV)]),
    ("w_br_attn", [DEPTH, HEADS * DV, D]), ("ssm_conv_w", [DEPTH, SCONV, CDIM]), ("ssm_conv_b", [DEPTH, CDIM]),
    ("ssm_dt_bias", [DEPTH, 32]), ("ssm_a_log", [DEPTH, 32]), ("ssm_d", [DEPTH, SH]), ("ssm_norm", [DEPTH, DI]),
    ("w_br_ssm", [DEPTH, DI, D]), ("cnv_dw_w", [DEPTH, CW, CCH]), ("cnv_dw_b", [DEPTH, CCH]),
    ("cnv_ln_g", [DEPTH, CCH]), ("cnv_ln_b", [DEPTH, CCH]), ("w_br_conv", [DEPTH, CCH, D]), ("b_br_conv", [DEPTH, D]),
    ("w_out", [DEPTH, D, D]), ("ln1_g", [DEPTH, D]), ("ln1_b", [DEPTH, D]), ("router_w", [DEPTH, D, NE]),
    ("router_b", [DEPTH, NE]), ("moe_w_gate_up", [DEPTH, NE, D, 2 * DFF]), ("moe_b_gate_up", [DEPTH, NE, 2 * DFF]),
    ("moe_w_down", [DEPTH, NE, DFF, D]), ("moe_b_down", [DEPTH, NE, D]), ("ln2_g", [DEPTH, D]), ("ln2_b", [DEPTH, D]),
]


def build_program(S, depth=DEPTH, debug=False, stop_after=99, skip=()):
    nc = bass.Bass("TRN2", target_bir_lowering=False)
    x = nc.dram_tensor("x", [S, D], F32, kind="ExternalInput").ap()
    W = {n: nc.dram_tensor(n, sh, F32, kind="ExternalInput").ap() for n, sh in PARAM_SHAPES}
    ropec = nc.dram_tensor("ropec", [S, 16], F32, kind="ExternalInput").ap()
    ropes = nc.dram_tensor("ropes", [S, 16], F32, kind="ExternalInput").ap()
    ropeT = nc.dram_tensor("ropeT", [2, 96, S], F32, kind="ExternalInput").ap()
    out = nc.dram_tensor("out", [S, D], F32, kind="ExternalOutput").ap()
    dk = "ExternalOutput" if debug else None

    def scr(name, shape, dt):
        if dk:
            return nc.dram_tensor(name, shape, dt, kind=dk).ap()
        return nc.dram_tensor(name, shape, dt).ap()
    tm_q = scr("tm_q", [S, 672], F32)
    tm_z = scr("tm_z", [S, 1024], F32)
    tm_dt = scr("tm_dt", [S, 32], F32)
    fm_xbc = scr("fm_xbc", [1536, S], BF16)
    fm_u = scr("fm_u", [512, S], BF16)
    fm_g = scr("fm_g", [3072, S], BF16)
    fm_xa = scr("fm_xa", [1536, S], BF16)
    fm_cv = scr("fm_cv", [512, S], BF16)
    fm_o = scr("fm_o", [512, S], BF16)
    fm_ys = scr("fm_ys", [1024, S], BF16)
    x1 = scr("x1", [S, D], F32)
    xmid = scr("xmid", [S, D], F32)
    Xg = scr("Xg", [NSLOT, D], BF16)
    Yg = scr("Yg", [NSLOT, D], F32)
    tm_slot = scr("tm_slot", [S, 4], I32)
    tm_gate = scr("tm_gate", [S, 4], F32)
    with contextlib.ExitStack() as st:
        P = Prog(nc, st)
        C = Ctx()
        setup_consts(nc, P, st, C)
        cur = x
        for l in range(depth):
            _LAYER[0] = l
            dst = out if l == depth - 1 else xmid
            phase_inproj(nc, P, C, S, cur, W["w_in"][l], W["b_in"][l], tm_q, tm_z, tm_dt, fm_xbc, fm_u, fm_g)
            if stop_after >= 2:
                phase_conv(nc, P, C, S, fm_xbc, fm_u, W["ssm_conv_w"][l], W["ssm_conv_b"][l], W["cnv_dw_w"][l],
                           W["cnv_dw_b"][l], W["cnv_ln_g"][l], W["cnv_ln_b"][l], fm_xa, fm_cv)
            if stop_after >= 3:
                phase_attn(nc, P, C, S, tm_q, W["mla_q_norm"][l], W["mla_kv_norm"][l], W["mla_w_uq"][l],
                           W["mla_w_ukv"][l], ropec, ropes, ropeT, fm_o, Xg=Xg)
            if stop_after >= 4 and 4 not in skip:
                phase_ssd(nc, P, C, S, fm_xa, tm_dt, tm_z, W["ssm_dt_bias"][l], W["ssm_a_log"][l], W["ssm_d"][l],
                          W["ssm_norm"][l], fm_ys)
            if stop_after >= 5 and 5 not in skip:
                phase_merge(nc, P, C, S, cur, fm_o, fm_ys, fm_cv, fm_g, W["w_br_attn"][l], W["w_br_ssm"][l],
                            W["w_br_conv"][l], W["b_br_conv"][l], W["w_out"][l], W["ln1_g"][l], W["ln1_b"][l],
                            W["router_w"][l], W["router_b"][l], x1, Xg, tm_slot, tm_gate)
            if stop_after >= 7 and 7 not in skip:
                phase_experts(nc, P, C, Xg, W["moe_w_gate_up"][l], W["moe_b_gate_up"][l], W["moe_w_down"][l],
                              W["moe_b_down"][l], Yg)
            if stop_after >= 8:
                phase_combine(nc, P, C, S, x1, Yg, tm_slot, tm_gate, W["ln2_g"][l], W["ln2_b"][l], dst)
            cur = dst
    return nc


def rope_tables(S):
    pos = np.arange(S, dtype=np.float32)
    inv = (np.float32(10000.0) ** (-np.arange(0, DR, 2, dtype=np.float32) / np.float32(DR))).astype(np.float32)
    ang = (pos[:, None] * inv[None, :]).astype(np.float32)
    c = np.cos(ang).astype(np.float32)
    s = np.sin(ang).astype(np.float32)
    rT = np.zeros((2, 96, S), np.float32)
    rT[0, 64:80] = c.T
    rT[0, 80:96] = c.T
    rT[1, 64:80] = s.T
    rT[1, 80:96] = s.T
    return c, s, rT


_NC_CACHE = {}


def kernel(**inputs):
    x = np.ascontiguousarray(inputs["x"], dtype=np.float32)
    B, S, _ = x.shape
    key = (S,)
    if key not in _NC_CACHE:
        _NC_CACHE[key] = build_program(S)
    nc = _NC_CACHE[key]
    c, s, rT = rope_tables(S)
    shared = {}
    for n, sh in PARAM_SHAPES:
        shared[n] = np.ascontiguousarray(inputs[n], dtype=np.float32).reshape(sh)
    shared.update(ropec=c, ropes=s, ropeT=rT)
    in_maps = [dict(shared, x=x[b]) for b in range(B)]
    res = run_bass_kernel_spmd(nc, in_maps, core_ids=list(range(B)))
    return np.stack([r["out"] for r in res.results], axis=0)
```
